# Optimizing a Trainium2 kernel written in Bass

```python
import jax, jax.numpy as jnp
from jax import lax
import numpy as np

D_MODEL = 1024
BATCH = 4
SEQ = 4096
DEPTH = 1

N_ATTN_HEADS = 8
HEAD_DIM = 64
ATTN_WIDTH = N_ATTN_HEADS * HEAD_DIM
POOL_WIDTH = D_MODEL - ATTN_WIDTH
POOL_WINDOWS = (2, 4, 8, 16)
N_POOL_GROUPS = len(POOL_WINDOWS)
POOL_GROUP = POOL_WIDTH // N_POOL_GROUPS
IN_WIDTH = 3 * ATTN_WIDTH + POOL_WIDTH
MOBA_BLOCK = 256
MOBA_TOPK = 3
QUERY_CHUNK = 32
D_FF = 4 * D_MODEL
EPS = 1e-6

kernel_name = "hymba_moba_pool_sqrelu_block"


def _alibi_slopes(n_heads):
    return jnp.asarray(2.0 ** (-8.0 * np.arange(1, n_heads + 1) / n_heads), dtype=jnp.float32)


def rms_norm(x, g):
    xf = x.astype(jnp.float32)
    y = xf * lax.rsqrt(jnp.mean(xf * xf, axis=-1, keepdims=True) + EPS)
    return (y * g.astype(jnp.float32)).astype(x.dtype)


def moba_attention(q, k, v):
    B, H, S, Dh = q.shape
    nb = -(-S // MOBA_BLOCK)
    pad = nb * MOBA_BLOCK - S
    kp = jnp.pad(k, ((0, 0), (0, 0), (0, pad), (0, 0)))
    vp = jnp.pad(v, ((0, 0), (0, 0), (0, pad), (0, 0)))
    kb = kp.reshape(B, H, nb, MOBA_BLOCK, Dh)
    vb = vp.reshape(B, H, nb, MOBA_BLOCK, Dh)
    slopes = _alibi_slopes(H)
    scale = HEAD_DIM ** -0.5

    counts = jnp.asarray(np.clip(S - np.arange(nb) * MOBA_BLOCK, 1, MOBA_BLOCK), dtype=jnp.float32)
    kmean = kb.astype(jnp.float32).sum(axis=3) / counts[:, None]
    pos = jnp.arange(S)
    qblk = pos // MOBA_BLOCK
    gate = jnp.einsum('bhtd,bhnd->bhtn', q.astype(jnp.float32), kmean)
    past = jnp.arange(nb)[None, :] < qblk[:, None]
    gate = jnp.where(past[None, None], gate, -jnp.inf)
    n_sel = max(1, min(MOBA_TOPK, nb - 1))
    _, sel = lax.top_k(gate, n_sel)
    sel_valid = jnp.arange(n_sel)[None, :] < qblk[:, None]

    nc = S // QUERY_CHUNK
    qc = q.reshape(B, H, nc, QUERY_CHUNK, Dh).transpose(2, 0, 1, 3, 4)
    selc = sel.reshape(B, H, nc, QUERY_CHUNK, n_sel).transpose(2, 0, 1, 3, 4)
    validc = sel_valid.reshape(nc, QUERY_CHUNK, n_sel)
    bi = jnp.arange(B)[:, None, None, None]
    hi = jnp.arange(H)[None, :, None, None]
    blk_off = jnp.arange(MOBA_BLOCK)

    def chunk(args):
        c, q_c, sel_c, valid_c = args
        t = c * QUERY_CHUNK + jnp.arange(QUERY_CHUNK)
        own = t[0] // MOBA_BLOCK
        k_own = lax.dynamic_index_in_dim(kb, own, axis=2, keepdims=False)
        v_own = lax.dynamic_index_in_dim(vb, own, axis=2, keepdims=False)
        s_own = own * MOBA_BLOCK + blk_off
        sc_own = jnp.einsum('bhtd,bhsd->bhts', q_c, k_own).astype(jnp.float32) * scale
        dist_own = (t[:, None] - s_own[None, :]).astype(jnp.float32)
        sc_own = sc_own - slopes[:, None, None] * dist_own[None]
        sc_own = jnp.where((s_own[None, :] <= t[:, None])[None, None], sc_own, -jnp.inf)

        k_sel = kb[bi, hi, sel_c]
        v_sel = vb[bi, hi, sel_c]
        s_sel = sel_c[..., None] * MOBA_BLOCK + blk_off
        sc_sel = jnp.einsum('bhtd,bhtrsd->bhtrs', q_c, k_sel).astype(jnp.float32) * scale
        dist_sel = (t[None, None, :, None, None] - s_sel).astype(jnp.float32)
        sc_sel = sc_sel - slopes[None, :, None, None, None] * dist_sel
        sc_sel = jnp.where(valid_c[None, None, :, :, None], sc_sel, -jnp.inf)

        scores = jnp.concatenate(
            [sc_own, sc_sel.reshape(B, H, QUERY_CHUNK, n_sel * MOBA_BLOCK)], axis=-1)
        p = jax.nn.softmax(scores, axis=-1)
        p_own = p[..., :MOBA_BLOCK].astype(v.dtype)
        p_sel = p[..., MOBA_BLOCK:].reshape(B, H, QUERY_CHUNK, n_sel, MOBA_BLOCK).astype(v.dtype)
        out = (jnp.einsum('bhts,bhsd->bhtd', p_own, v_own)
               + jnp.einsum('bhtrs,bhtrsd->bhtd', p_sel, v_sel))
        return out.astype(q.dtype)

    out = lax.map(chunk, (jnp.arange(nc), qc, selc, validc))
    return out.transpose(1, 2, 0, 3, 4).reshape(B, H, S, Dh)


def multiscale_pool(u, w_pool, pool_scale):
    B, S, _ = u.shape
    ug = u.astype(jnp.float32).reshape(B, S, N_POOL_GROUPS, POOL_GROUP)
    cs = jnp.cumsum(ug, axis=1)
    pos = jnp.arange(S)
    outs = []
    for g, w in enumerate(POOL_WINDOWS):
        c = cs[:, :, g]
        prev = jnp.pad(c, ((0, 0), (w, 0), (0, 0)))[:, :S]
        cnt = jnp.minimum(pos + 1, w).astype(jnp.float32)[None, :, None]
        outs.append((c - prev) / cnt - ug[:, :, g])
    mixed = jnp.stack(outs, axis=2)
    y = jnp.einsum('bsgc,gcd->bsgd', mixed, w_pool.astype(jnp.float32))
    y = y.reshape(B, S, POOL_WIDTH) * pool_scale.astype(jnp.float32)
    return y.astype(u.dtype)


def setup_inputs(seed: int = 0) -> dict:
    key = jax.random.key(seed)
    ks = jax.random.split(key, 12)
    f32 = jnp.float32
    x = jax.random.normal(ks[0], (BATCH, SEQ, D_MODEL), f32)
    norm_mix = 1.0 + 0.02 * jax.random.normal(ks[1], (DEPTH, D_MODEL), f32)
    w_in = jax.random.normal(ks[2], (DEPTH, D_MODEL, IN_WIDTH), f32) * D_MODEL ** -0.5
    w_pool = jax.random.normal(ks[3], (DEPTH, N_POOL_GROUPS, POOL_GROUP, POOL_GROUP), f32) * POOL_GROUP ** -0.5
    pool_scale = 1.0 + 0.02 * jax.random.normal(ks[4], (DEPTH, POOL_WIDTH), f32)
    w_out = jax.random.normal(ks[5], (DEPTH, D_MODEL, D_MODEL), f32) * D_MODEL ** -0.5
    norm_mlp = 1.0 + 0.02 * jax.random.normal(ks[6], (DEPTH, D_MODEL), f32)
    w_up = jax.random.normal(ks[7], (DEPTH, D_MODEL, D_FF), f32) * D_MODEL ** -0.5
    w_down = jax.random.normal(ks[8], (DEPTH, D_FF, D_MODEL), f32) * D_FF ** -0.5
    norm_final = 1.0 + 0.02 * jax.random.normal(ks[9], (D_MODEL,), f32)
    return {"x": x, "norm_mix": norm_mix, "w_in": w_in, "w_pool": w_pool,
            "pool_scale": pool_scale, "w_out": w_out, "norm_mlp": norm_mlp,
            "w_up": w_up, "w_down": w_down, "norm_final": norm_final}


def reference(x, norm_mix, w_in, w_pool, pool_scale, w_out, norm_mlp, w_up, w_down, norm_final):
    B, S, _ = x.shape
    for l in range(DEPTH):
        h = rms_norm(x, norm_mix[l])
        proj = h @ w_in[l]
        q, k, v, u = jnp.split(proj, [ATTN_WIDTH, 2 * ATTN_WIDTH, 3 * ATTN_WIDTH], axis=-1)
        to_heads = lambda t: t.reshape(B, S, N_ATTN_HEADS, HEAD_DIM).transpose(0, 2, 1, 3)
        a = moba_attention(to_heads(q), to_heads(k), to_heads(v))
        a = a.transpose(0, 2, 1, 3).reshape(B, S, ATTN_WIDTH)
        p = multiscale_pool(u, w_pool[l], pool_scale[l])
        x = x + jnp.concatenate([a, p], axis=-1) @ w_out[l]
        h = rms_norm(x, norm_mlp[l])
        x = x + jnp.square(jax.nn.relu(h @ w_up[l])) @ w_down[l]
    return rms_norm(x, norm_final)
```

```python
import numpy as np
import ml_dtypes
from contextlib import ExitStack
import concourse.bass as bass
import concourse.mybir as mybir
from concourse.bass_utils import run_bass_kernel_spmd

F32 = mybir.dt.float32
BF16 = mybir.dt.bfloat16
ALU = mybir.AluOpType
AF = mybir.ActivationFunctionType

ENGS = ("pe", "act", "dve", "pool", "sp")


class Op:
    __slots__ = ("eng", "fn", "reads", "writes", "dma", "idx", "deps", "waits",
                 "signal", "dom", "val", "clock", "extra")

    def __init__(self, eng, fn, reads, writes, dma):
        self.eng = eng
        self.fn = fn
        self.reads = tuple(reads)
        self.writes = tuple(writes)
        self.dma = dma
        self.signal = False
        self.waits = []
        self.deps = []
        self.extra = []
        self.idx = 0
        self.val = 0


class Prog:
    def __init__(self, nc, n_dma_sems=20):
        self.nc = nc
        self.ops = []
        self.n_dma_sems = n_dma_sems

    def op(self, eng, fn, reads=(), writes=(), dma=False, after=()):
        o = Op(eng, fn, reads, writes, dma)
        o.extra = list(after)
        self.ops.append(o)
        return o

    def _analyze(self):
        last_write = {}
        reads_since = {}
        eng_count = {e: 0 for e in ENGS}
        dma_rr = {e: 0 for e in ENGS}
        dma_last = {}
        dma_val = {}
        for o in self.ops:
            if o.dma:
                k = dma_rr[o.eng] % self.n_dma_sems
                dma_rr[o.eng] += 1
                o.dom = ("dma", o.eng, k)
                o.val = dma_val.get(o.dom, 0) + 16
                dma_val[o.dom] = o.val
                prev = dma_last.get(o.dom)
                if prev is not None:
                    o.deps.append(prev)
                dma_last[o.dom] = o
            else:
                o.dom = o.eng
                eng_count[o.eng] += 1
                o.idx = eng_count[o.eng]
            deps = list(o.extra)
            for b in o.reads:
                lw = last_write.get(b)
                if lw:
                    deps.extend(lw.values())
            for b in o.writes:
                lw = last_write.get(b)
                if lw:
                    deps.extend(lw.values())
                rs = reads_since.get(b)
                if rs:
                    deps.extend(rs.values())
            seen = set()
            for d in deps:
                if d is o or id(d) in seen:
                    continue
                seen.add(id(d))
                if (not d.dma) and (not o.dma) and d.eng == o.eng:
                    if o.eng == "pe":
                        continue
                o.deps.append(d)
            for b in o.reads:
                reads_since.setdefault(b, {})[o.dom] = o
            for b in o.writes:
                last_write[b] = {o.dom: o}
                reads_since[b] = {}
        know = {e: {} for e in ENGS}
        for o in self.ops:
            kn = know[o.eng]
            for d in o.deps:
                dv = d.val if d.dma else d.idx
                if (not d.dma) and d.eng == o.eng:
                    key = ("self", d.dom)
                    if kn.get(key, 0) >= dv:
                        continue
                    kn[key] = dv
                    o.waits.append(d)
                    d.signal = True
                    continue
                if kn.get(d.dom, 0) >= dv:
                    continue
                o.waits.append(d)
                d.signal = True
                kn[d.dom] = dv
                for k2, v2 in d.clock.items():
                    if isinstance(k2, tuple) and k2[0] == "self":
                        continue
                    if kn.get(k2, 0) < v2:
                        kn[k2] = v2
            o.clock = dict(kn)
            if not o.dma:
                o.clock[o.dom] = o.idx
        cnt = {e: 0 for e in ENGS}
        for o in self.ops:
            if o.dma:
                o.signal = True
                continue
            if o.signal:
                cnt[o.eng] += 1
                o.val = cnt[o.eng]

    def emit(self, stack):
        nc = self.nc
        self._analyze()
        sems = {}
        for e in ENGS:
            sems[e] = stack.enter_context(nc.semaphore("s_" + e))
        for o in self.ops:
            if o.dma and o.dom not in sems:
                sems[o.dom] = stack.enter_context(
                    nc.semaphore("d_%s_%d" % (o.dom[1], o.dom[2])))
        block = stack.enter_context(nc.Block())
        by_eng = {e: [o for o in self.ops if o.eng == e] for e in ENGS}

        def run(eng_name):
            def body(eng):
                for o in by_eng[eng_name]:
                    for d in o.waits:
                        eng.wait_ge(sems[d.dom], d.val)
                    ins = o.fn(eng)
                    if o.signal and ins is not None:
                        ins.then_inc(sems[o.dom], 16 if o.dma else 1)
            return body

        block.tensor(run("pe"))
        block.scalar(run("act"))
        block.vector(run("dve"))
        block.gpsimd(run("pool"))
        block.sync(run("sp"))
        return dict(n_ops=len(self.ops), n_waits=sum(len(o.waits) for o in self.ops),
                    per_eng={e: len(by_eng[e]) for e in ENGS})


D = 1024
SEQ = 4096
NBATCH = 4
H = 8
DH = 64
BLK = 256
NBLK = 16
NOWN = 8
NTOK = NOWN * BLK
NT = NTOK // 128
DFF = 4096
EPS = 1e-6
BIG = 30000.0
ALIBI_CUT = 45.0
WINS = (2, 4, 8, 16)
SLOPES = [2.0 ** (-(h + 1)) for h in range(H)]
NCORES = 8


def build_program(debug=False):
    nc = bass.Bass("TRN2", target_bir_lowering=False)

    def din(name, shape, dt=F32):
        return nc.dram_tensor(name, shape, dt, kind="ExternalInput").ap()

    x_own = din("x_own", [NTOK, D])
    x_ctx = din("x_ctx", [NTOK, D])
    x_hist = din("x_hist", [128, D])
    w_in = din("w_in", [D, 2048])
    w_out = din("w_out", [D, D])
    w_up = din("w_up", [D, DFF])
    w_down = din("w_down", [DFF, D])
    w_pool = din("w_pool", [512, 128])
    pscale_t = din("pscale_t", [128, 4])
    gmix_t = din("gmix_t", [128, 8])
    g_mlp = din("g_mlp", [D])
    g_fin = din("g_fin", [D])
    kaug = din("kaug", [20, 4096], BF16)
    qaug = din("qaug", [4, H * NTOK], BF16)
    elig_d = din("elig", [128, NOWN * 16], BF16)
    owntab_d = din("owntab", [128, NOWN * 16], BF16)
    tri_d = din("tri", [128, 128], BF16)
    ident_d = din("ident", [128, 128], BF16)
    cntfix_d = din("cntfix", [128, 64])
    y = nc.dram_tensor("y", [NTOK, D], F32, kind="ExternalOutput").ap()
    dbg = {}
    if debug:
        dbg["cat"] = nc.dram_tensor("dbg_cat", [128, 8 * NTOK], BF16, kind="ExternalOutput").ap()
        dbg["qt"] = nc.dram_tensor("dbg_qt", [128, 8 * NTOK], BF16, kind="ExternalOutput").ap()
        dbg["kt"] = nc.dram_tensor("dbg_kt", [128, 8 * 4096], BF16, kind="ExternalOutput").ap()
        dbg["vp"] = nc.dram_tensor("dbg_vp", [128, 32 * 8 * 65], BF16, kind="ExternalOutput").ap()

    with ExitStack() as st:
        RA = st.enter_context(nc.sbuf_tensor("RA", [128, 32768], BF16))
        RBC = st.enter_context(nc.sbuf_tensor("RBC", [128, 33024], BF16))
        RD = st.enter_context(nc.sbuf_tensor("RD", [128, 16384], BF16))
        RE = st.enter_context(nc.sbuf_tensor("RE", [128, 8192], BF16))
        NW = 16032
        RW = st.enter_context(nc.sbuf_tensor("RW", [128, NW], BF16))
        psb = [st.enter_context(nc.psum_tensor("ps%d" % b, [128, 512], F32)) for b in range(8)]

        KT = RA[:, :].rearrange("p (h s) -> p h s", h=8)
        WST = RA[:, :].bitcast(F32).rearrange("p (k n) -> p k n", k=8)
        XM = RA[:, :].bitcast(F32).rearrange("p (t d) -> p t d", t=16)
        VP = RBC[:, 0:16640].rearrange("p (c h e) -> p c h e", c=32, h=8)
        VP1 = RBC[:, 0:16640].rearrange("p (m e) -> p m e", e=65)
        QT = RBC[:, 16640:33024].rearrange("p (h s) -> p h s", h=8)
        CAT = RD[:, :].rearrange("p (c s) -> p c s", c=8)
        WQK = RD[:, 0:8192].rearrange("p (k n) -> p k n", k=8)
        WVU = RE[:, :].rearrange("p (k n) -> p k n", k=8)
        WOUT = WVU

        def ring(slot):
            base = slot * 16384
            wu = RBC[:, base:base + 8192].rearrange("p (k n) -> p k n", k=8)
            wd = RBC[:, base + 8192:base + 16384].rearrange("p (k n) -> p k n", k=8)
            return wu, wd

        class Carver:
            def __init__(self):
                self.off = 0

            def bf(self, n):
                a = RW[:, self.off:self.off + n]
                self.off += n
                assert self.off <= NW, self.off
                return a

            def f32(self, n):
                if self.off % 2:
                    self.off += 1
                a = RW[:, self.off:self.off + 2 * n].bitcast(F32)
                self.off += 2 * n
                assert self.off <= NW, self.off
                return a

        cv = Carver()
        tri = cv.bf(128)
        ident = cv.bf(128)
        ss = cv.f32(68)
        rs = cv.f32(68)
        dummy = cv.f32(16)
        epsb = cv.f32(2)
        elig = cv.bf(128).rearrange("p (i s) -> p i s", i=8)
        owntab = cv.bf(128).rearrange("p (i s) -> p i s", i=8)
        kmT = cv.bf(128)
        persist_end = cv.off

        cntfix = cv.f32(64)
        wpool = cv.bf(512).rearrange("p (g d) -> p g d", g=4)
        pscale = cv.f32(4)
        cv_pscw = cv.f32(4)
        gcol = cv.f32(8)
        ksum = cv.f32(128)
        fx = cv.f32(16)
        uH = cv.f32(512).rearrange("p (g s) -> p g s", g=4)
        ub = [cv.f32(272), cv.f32(272)]
        sS = [cv.f32(272) for _ in range(3)]
        mixT = [cv.bf(256) for _ in range(4)]
        hb = cv.bf(1024)
        hTb = [cv.bf(2048).rearrange("p (k s) -> p k s", k=8) for _ in range(2)]
        xt = [cv.f32(1024), cv.f32(1024)]
        p1_end = cv.off

        P = Prog(nc)

        def fb_(bank):
            return [("ps", bank)]
        dummy_i = [0]

        def fence(old_ids, new_ids):
            j = dummy_i[0] % 16
            dummy_i[0] += 1
            return P.op("pool", lambda e: e.memset(dummy[0:1, j:j + 1], 0.0),
                        writes=list(old_ids) + list(new_ids) + [("fx_dummy", j)])

        P.op("sp", lambda e: e.dma_start(out=tri, in_=tri_d), writes=["tri"], dma=True)
        P.op("sp", lambda e: e.dma_start(out=ident, in_=ident_d), writes=["ident"], dma=True)
        P.op("sp", lambda e: e.dma_start(out=gcol, in_=gmix_t), writes=["gcol"], dma=True)
        w_in_v = w_in.rearrange("(k p) n -> p k n", p=128)
        WGRP = [("wu", 1536, WVU, 512), ("wk", 512, WQK, 512), ("wv", 1024, WVU, 0), ("wq", 0, WQK, 0)]
        P.op("sp", lambda e: e.dma_start(out=cntfix, in_=cntfix_d), writes=["cntfix"], dma=True)
        P.op("sp", lambda e: e.dma_start(out=elig.rearrange("p i s -> p (i s)"), in_=elig_d), writes=["elig"], dma=True)
        P.op("sp", lambda e: e.dma_start(out=owntab.rearrange("p i s -> p (i s)"), in_=owntab_d), writes=["owntab"], dma=True)
        P.op("sp", lambda e: e.dma_start(out=pscale, in_=pscale_t), writes=["pscale"], dma=True)
        P.op("pool", lambda e: e.dma_start(out=wpool, in_=w_pool.rearrange("(g c) d -> c g d", g=4)),
             writes=["wpool"], dma=True)
        P.op("sp", lambda e: e.dma_start(out=QT[80:84, :, :], in_=qaug.rearrange("r (h s) -> r h s", h=8)),
             writes=["qaug"], dma=True)
        P.op("pool", lambda e: e.memset(VP1[:, :, 64:65], 1.0), writes=["vones"])
        P.op("pool", lambda e: e.memset(ksum, 0.0), writes=[("ksum", h, ks) for h in range(8) for ks in range(16)])
        P.op("pool", lambda e: e.memset(kmT, 0.0), writes=[("kmT", ks) for ks in range(16)])
        P.op("pool", lambda e: e.memset(ss, 0.0), writes=[("ss", c) for c in range(68)])
        P.op("pool", lambda e: e.memset(epsb, EPS), writes=["epsb"])

        pscw = cv_pscw
        for g in range(4):
            P.op("dve", lambda e, g=g: e.tensor_scalar(out=pscw[:, g:g + 1], in0=pscale[:, g:g + 1],
                                                      scalar1=1.0 / WINS[g], scalar2=None, op0=ALU.mult),
                 reads=["pscale"], writes=["pscw"])
        kt_ids = [("kT", h, ks) for h in range(8) for ks in range(16)] + [("kaug", h) for h in range(8)]
        tile_ctr = [0]
        blk_ctr = [0]
        pp_ctr = [0]
        pv_ctr = [0]
        tr_ctr = [0]
        ub_ctr = [0]
        mix_ctr = [0]

        def pp_slot():
            n = pp_ctr[0] % 4
            pp_ctr[0] += 1
            bank = 2 + n
            return psb[bank][:, 0:256], ("ps", bank)

        def tr_bank():
            n = tr_ctr[0] % 2
            tr_ctr[0] += 1
            return psb[n][:, :].bitcast(BF16), fb_(n)

        def norm_rs(col, src_ap, src_ids, junk, junk_id):
            P.op("act", lambda e: e.activation(out=junk, in_=src_ap, func=AF.Square, accum_out=ss[:, col:col + 1]),
                 reads=src_ids, writes=[junk_id, ("ss", col)])
            P.op("act", lambda e: e.activation(out=rs[:, col:col + 1], in_=ss[:, col:col + 1], func=AF.Ln,
                                               scale=1.0 / D, bias=epsb[:, 0:1]),
                 reads=[("ss", col), "epsb"], writes=[("rs", col)])
            P.op("act", lambda e: e.activation(out=rs[:, col:col + 1], in_=rs[:, col:col + 1], func=AF.Exp, scale=-0.5),
                 reads=[("rs", col)], writes=[("rs", col)])

        def a_begin(kind, i):
            bidx = blk_ctr[0]
            blk_ctr[0] += 1
            return bidx % 2

        def a_tile(kind, i, buf, tt, split=False):
            if kind == "hist" and tt == 1:
                return
            if True:
                tc_ = tile_ctr[0]
                tile_ctr[0] += 1
                slot = tc_ % 2
                col = tc_
                if kind == "hist":
                    src = x_hist
                elif kind == "ctx":
                    src = x_ctx[(i * 2 + tt) * 128:(i * 2 + tt + 1) * 128, :]
                else:
                    src = x_own[(i * 2 + tt) * 128:(i * 2 + tt + 1) * 128, :]
                xts = xt[slot]
                P.op("sp", lambda e, xts=xts, src=src: e.dma_start(out=xts, in_=src), writes=[("xt", slot)], dma=True)
                norm_rs(col, xts, [("xt", slot)], hb, "hb")
                P.op("dve", lambda e, xts=xts, col=col: e.tensor_scalar(out=hb, in0=xts, scalar1=rs[:, col:col + 1],
                                                                       scalar2=None, op0=ALU.mult),
                     reads=[("xt", slot), ("rs", col)], writes=["hb"])

                def a_trans(buf=buf, tt=tt):
                    trv, trid = tr_bank()
                    for kc in range(8):
                        P.op("pe", lambda e, trv=trv, kc=kc: e.transpose(out=trv[:, kc * 128:(kc + 1) * 128],
                                                                         in_=hb[:, kc * 128:(kc + 1) * 128], identity=ident),
                             reads=["hb", "ident"], writes=trid)
                    P.op("dve", lambda e, trv=trv: e.tensor_copy(
                        out=hTb[buf][:, :, tt * 128:(tt + 1) * 128], in_=trv.rearrange("p (k s) -> p k s", k=8)),
                        reads=trid, writes=[("hT", buf, tt)])
                if split:
                    return a_trans
                a_trans()

        def proj(buf, wview, wname, c0, ntok):
            pv, pid = pp_slot()
            for kc in range(8):
                P.op("pe", lambda e, pv=pv, kc=kc: e.matmul(out=pv[:, 0:ntok], lhsT=wview[:, kc, c0:c0 + 128],
                                                            rhs=hTb[buf][:, kc, 0:ntok], start=(kc == 0), stop=(kc == 7)),
                     reads=[(wname, kc), ("hT", buf, 0), ("hT", buf, 1)], writes=[pid])
            return pv, pid

        deferred = []
        GMH = [("gmh", h) for h in range(8)]

        def flush_deferred():
            while deferred:
                deferred.pop(0)()

        gps_state = {}

        def stage_b1(kind, i, buf):
            if kind == "hist":
                for g in range(4):
                    pv, pid = proj(buf, WVU, "wu", 512 + g * 128, 128)
                    P.op("dve", lambda e, pv=pv, g=g: e.tensor_copy(out=uH[:, g, :], in_=pv[:, 0:128]),
                         reads=[pid], writes=[("uH", g)])
                return
            ks = 2 * i if kind == "ctx" else 2 * i + 1
            for cg in range(4):
                pv, pid = proj(buf, WQK, "wk", 512 + cg * 128, 256)
                for half in range(2):
                    h = 2 * cg + half
                    P.op("act", lambda e, pv=pv, h=h, half=half: e.activation(
                        out=KT[0:64, h, ks * 256:(ks + 1) * 256], in_=pv[half * 64:(half + 1) * 64, 0:256],
                        func=AF.Copy, accum_out=ksum[0:64, h * 16 + ks:h * 16 + ks + 1]),
                        reads=[pid], writes=[("kT", h, ks), ("ksum", h, ks)])
            ksv = ksum.rearrange("p (h s) -> p h s", h=8)
            kmv = kmT.rearrange("p (h s) -> p h s", h=8)
            P.op("dve", lambda e: e.tensor_scalar(out=kmv[0:64, :, ks:ks + 1], in0=ksv[0:64, :, ks:ks + 1],
                                                  scalar1=1.0 / BLK, scalar2=None, op0=ALU.mult),
                 reads=[("ksum", h, ks) for h in range(8)], writes=[("kmT", ks)])

        def stage_b1v(kind, i, buf):
            if kind == "hist":
                return
            ks = 2 * i if kind == "ctx" else 2 * i + 1
            for tt in range(2):
                n = pv_ctr[0] % 2
                pv_ctr[0] += 1
                pvv = psb[6 + n]
                pvid = fb_(6 + n)
                for kc in range(8):
                    P.op("pe", lambda e, pvv=pvv, kc=kc, tt=tt: e.matmul(
                        out=pvv[:, :], lhsT=hTb[buf][:, kc, tt * 128:(tt + 1) * 128], rhs=WVU[:, kc, 0:512],
                        start=(kc == 0), stop=(kc == 7)),
                        reads=[("wv", kc), ("hT", buf, tt)], writes=pvid)
                chunk = ks * 2 + tt
                P.op("dve", lambda e, pvv=pvv, chunk=chunk: e.tensor_copy(
                    out=VP[:, chunk, :, 0:64], in_=pvv[:, :].rearrange("p (h d) -> p h d", h=8)),
                    reads=pvid, writes=[("V", chunk)])
            flush_deferred()

        def stage_b2(kind, i, buf):
            if kind != "own":
                return
            for cg in range(4):
                pv, pid = proj(buf, WQK, "wq", cg * 128, 256)
                for half in range(2):
                    h = 2 * cg + half
                    P.op("dve", lambda e, pv=pv, h=h, half=half: e.tensor_scalar(
                        out=QT[0:64, h, i * 256:(i + 1) * 256], in0=pv[half * 64:(half + 1) * 64, 0:256],
                        scalar1=DH ** -0.5, scalar2=None, op0=ALU.mult),
                        reads=[pid], writes=[("qT", h, i)])
            combines = []
            for g in range(4):
                pv, pid = proj(buf, WVU, "wu", 512 + g * 128, 256)
                us = ub_ctr[0] % 2
                ub_ctr[0] += 1
                u_ = ub[us]
                uid = ("ub", us)
                P.op("dve", lambda e, pv=pv, u_=u_: e.tensor_copy(out=u_[:, 16:272], in_=pv[:, 0:256]),
                     reads=[pid], writes=[uid])
                P.op("pool", lambda e, u_=u_, g=g: e.tensor_copy(out=u_[:, 0:16], in_=uH[:, g, i * 16:(i + 1) * 16]),
                     reads=[("uH", g)], writes=[(uid, "h")])
                rd = [uid, (uid, "h")]
                if combines:
                    combines.pop(0)()
                sbuf = [sS[g % 3], sS[(g + 1) % 3]]
                sids = [("sS", g % 3), ("sS", (g + 1) % 3)]
                P.op("pool", lambda e, u_=u_, o_=sbuf[0]: e.tensor_tensor(out=o_[:, 1:272], in0=u_[:, 1:272], in1=u_[:, 0:271], op=ALU.add),
                     reads=rd, writes=[sids[0]])
                cur = 0
                lo = 1
                for lvl in range(g):
                    sh = 2 << lvl
                    nlo = lo + sh
                    src_, dst_ = sbuf[cur], sbuf[1 - cur]
                    P.op("pool", lambda e, src_=src_, dst_=dst_, nlo=nlo, sh=sh: e.tensor_tensor(
                        out=dst_[:, nlo:272], in0=src_[:, nlo:272], in1=src_[:, nlo - sh:272 - sh], op=ALU.add),
                        reads=[sids[cur]], writes=[sids[1 - cur]])
                    cur = 1 - cur
                    lo = nlo
                S = sbuf[cur]
                sid = sids[cur]
                mx = mixT[g]
                mid = ("mix", g)

                def combine(g=g, S=S, sid=sid, u_=u_, rd=rd, mx=mx, mid=mid):
                    P.op("dve", lambda e: e.scalar_tensor_tensor(
                        out=mx, in0=u_[:, 16:272], scalar=-float(WINS[g]), in1=S[:, 16:272], op0=ALU.mult, op1=ALU.add),
                        reads=[sid] + rd, writes=[mid])
                    if i == 0:
                        P.op("dve", lambda e: e.tensor_tensor(out=fx[:, 0:16], in0=S[:, 16:32],
                                                              in1=cntfix[:, g * 16:(g + 1) * 16], op=ALU.mult),
                             reads=[sid, "cntfix"], writes=["fx"])
                        P.op("dve", lambda e: e.scalar_tensor_tensor(
                            out=mx[:, 0:16], in0=u_[:, 16:32], scalar=-float(WINS[g]), in1=fx[:, 0:16],
                            op0=ALU.mult, op1=ALU.add),
                            reads=["fx", mid] + rd, writes=[mid])
                combines.append(combine)

                def pool_tail(g=g, mx=mx, mid=mid):
                    pv2, pid2 = pp_slot()
                    P.op("pe", lambda e: e.matmul(out=pv2[:, 0:256], lhsT=wpool[:, g, :], rhs=mx, start=True, stop=True),
                         reads=["wpool", mid], writes=[pid2])
                    P.op("dve", lambda e: e.tensor_scalar(out=CAT[:, 4 + g, i * 256:(i + 1) * 256], in0=pv2[:, 0:256],
                                                          scalar1=pscw[:, g:g + 1], scalar2=None, op0=ALU.mult),
                         reads=[pid2, "pscw"], writes=[("cat", 4 + g, 2 * i), ("cat", 4 + g, 2 * i + 1)])
                deferred.append(pool_tail)
            while combines:
                combines.pop(0)()

        order = [("hist", 0)]
        for i in range(NOWN):
            order += [("ctx", i), ("own", i)]
        bufs = {}
        bufs[0] = a_begin(*order[0])
        a_tile(order[0][0], order[0][1], bufs[0], 0)
        bufs[1] = a_begin(*order[1])
        a_tile(order[1][0], order[1][1], bufs[1], 0)
        for gi, (name, c0, dst, d0) in enumerate(WGRP):
            P.op(("sp", "act")[gi % 2], lambda e, c0=c0: e.dma_start(out=WST[:, :, c0:c0 + 512], in_=w_in_v[:, :, c0:c0 + 512]),
                 writes=[("wst", name)], dma=True)
        ci = 0
        for name, c0, dst, d0 in WGRP:
            for kc in range(8):
                eng = "dve"
                ci += 1
                src = WST[:, kc, c0:c0 + 512]
                if eng == "act":
                    fn = lambda e, dst=dst, kc=kc, src=src, d0=d0: e.activation(
                        out=dst[:, kc, d0:d0 + 512], in_=src, func=AF.Copy, scale=gcol[:, kc:kc + 1])
                else:
                    fn = lambda e, dst=dst, kc=kc, src=src, d0=d0: e.tensor_scalar(
                        out=dst[:, kc, d0:d0 + 512], in0=src, scalar1=gcol[:, kc:kc + 1], scalar2=None, op0=ALU.mult)
                P.op(eng, fn, reads=[("wst", name), "gcol"], writes=[(name, kc)])
        fence([("wst", name) for name, _, _, _ in WGRP], kt_ids)
        for h in range(8):
            P.op("pool", lambda e, h=h: e.dma_start(out=KT[64:84, h, :], in_=kaug), writes=[("kaug", h)], dma=True)
        for n in range(len(order)):
            nxt = order[n + 1] if n + 1 < len(order) else None
            if nxt and n > 0:
                bufs[n + 1] = a_begin(*nxt)
                a_tile(nxt[0], nxt[1], bufs[n + 1], 0)
            late = a_tile(nxt[0], nxt[1], bufs[n + 1], 1, split=True) if nxt else None
            stage_b1(order[n][0], order[n][1], bufs[n])
            if late:
                late()
            stage_b1v(order[n][0], order[n][1], bufs[n])
            stage_b2(order[n][0], order[n][1], bufs[n])
        flush_deferred()

        p1_ids = (["hb", "fx", "cntfix", "wpool", "pscale", "pscw", "gcol"]
                  + [("sS", n_) for n_ in range(3)] + [("mix", g) for g in range(4)]
                  + [("xt", s) for s in range(2)] + [("hT", b, t) for b in range(2) for t in range(2)]
                  + [("ub", s) for s in range(2)] + [(("ub", s), "h") for s in range(2)]
                  + [("uH", g) for g in range(4)]
                  + [("ksum", h, ks) for h in range(8) for ks in range(16)]
                  + [(w_, kc) for w_ in ("wq", "wk", "wv", "wu") for kc in range(8)])
        cv.off = persist_end
        PT = [cv.bf(512) for _ in range(4)]
        atok = [cv.bf(1024).rearrange("p (t c) -> p t c", t=2) for _ in range(2)]
        rec = [cv.f32(2) for _ in range(2)]
        gm = cv.f32(128)
        m8 = cv.f32(64)
        stage = [cv.bf(128), cv.bf(128)]
        p3_ids = ([("PT", n) for n in range(4)] + [("atok", n) for n in range(2)] + [("rec", n) for n in range(2)]
                  + [("gmh", h) for h in range(8)] + [("m8", h) for h in range(8)] + [("stage", t_) for t_ in range(2)]
                  + [("cat", c, t) for c in range(4) for t in range(NT)] + [("wout", kc) for kc in range(8)])
        fence(p1_ids, p3_ids)
        for kc in range(8):
            P.op("pool", lambda e, kc=kc: e.dma_start(out=WOUT[:, kc, :], in_=w_out[kc * 128:(kc + 1) * 128, :]),
                 writes=[("wout", kc)], dma=True)

        units = []
        for i in range(NOWN):
            for h in range(H):
                wmax = int(np.floor((ALIBI_CUT / SLOPES[h] - 1.0) / 256.0 - 1e-9))
                for u in range(2 * i + 1):
                    gap = 2 * i - u
                    if gap <= wmax:
                        units.append((i, h, u))
                units.append((i, h, "diag"))
        st_ctr = [0]
        po_state = {}

        def emit_qk(unit):
            i, h, u = unit
            n = st_ctr[0] % 4
            nb_ = st_ctr[0] % 3
            st_ctr[0] += 1
            stv = psb[nb_]
            sid = fb_(nb_)
            qsl = QT[0:84, h, i * 256:(i + 1) * 256]
            qreads = [("qT", h, i), ("qM", 2 * i), ("qM", 2 * i + 1), "qaug"]
            if u != "diag":
                ks = u
                for c in range(2):
                    P.op("pe", lambda e, c=c: e.matmul(out=stv[:, c * 256:(c + 1) * 256],
                                                       lhsT=KT[0:84, h, ks * 256 + c * 128:ks * 256 + (c + 1) * 128],
                                                       rhs=qsl, start=True, stop=True),
                         reads=[("kT", h, ks), ("kaug", h)] + qreads, writes=sid)
                width = 512
            else:
                ks = 2 * i + 1
                k0 = KT[0:84, h, ks * 256:ks * 256 + 128]
                k1 = KT[0:84, h, ks * 256 + 128:ks * 256 + 256]
                rd = [("kT", h, ks), ("kaug", h)] + qreads
                P.op("pe", lambda e: e.matmul(out=stv[:, 0:128], lhsT=k0, rhs=qsl[:, 0:128], start=True, stop=False),
                     reads=rd, writes=sid)
                P.op("pe", lambda e: e.matmul(out=stv[:, 0:128], lhsT=ident, rhs=tri, start=False, stop=True),
                     reads=["ident", "tri"], writes=sid)
                P.op("pe", lambda e: e.matmul(out=stv[:, 128:256], lhsT=k0, rhs=qsl[:, 128:256], start=True, stop=True),
                     reads=rd, writes=sid)
                P.op("pe", lambda e: e.matmul(out=stv[:, 256:384], lhsT=k1, rhs=qsl[:, 128:256], start=True, stop=False),
                     reads=rd, writes=sid)
                P.op("pe", lambda e: e.matmul(out=stv[:, 256:384], lhsT=ident, rhs=tri, start=False, stop=True),
                     reads=["ident", "tri"], writes=sid)
                width = 384
            ptv = PT[n]
            P.op("act", lambda e: e.activation(out=ptv[:, 0:width], in_=stv[:, 0:width], func=AF.Exp),
                 reads=sid, writes=[("PT", n)])
            return n

        def emit_pv(unit, n):
            i, h, u = unit
            key = (i, h)
            if key not in po_state:
                b = len(po_state) % 2
                po_state[key] = dict(bank=b, started=False)
            stt = po_state[key]
            b = stt["bank"]
            pov = psb[4 + b]
            poid = fb_(4 + b)
            ptv = PT[n]
            if u != "diag":
                ks = u
                jobs = [(0, 0, ks * 2), (1, 128, ks * 2), (0, 256, ks * 2 + 1), (1, 384, ks * 2 + 1)]
                last = [False] * 4
            else:
                ks = 2 * i + 1
                jobs = [(0, 0, ks * 2), (1, 128, ks * 2), (1, 256, ks * 2 + 1)]
                last = [False, False, True]
            for (tt, c0, chunk), lst in zip(jobs, last):
                first = not stt["started"]
                stt["started"] = True
                P.op("pe", lambda e, tt=tt, c0=c0, chunk=chunk, first=first, lst=lst: e.matmul(
                    out=pov[:, tt * 65:(tt + 1) * 65], lhsT=ptv[:, c0:c0 + 128], rhs=VP[:, chunk, h, :],
                    start=first, stop=lst),
                    reads=[("PT", n), ("V", chunk), "vones"], writes=poid)
            if u == "diag":
                ab = i % 2
                rc = rec[b]
                P.op("dve", lambda e: e.reciprocal(out=rc, in_=pov[:, 0:130].rearrange("p (t c) -> p t c", t=2)[:, :, 64]),
                     reads=poid, writes=[("rec", b)])
                for tt in range(2):
                    P.op("dve", lambda e, tt=tt: e.tensor_scalar(
                        out=atok[ab][:, tt, h * 64:(h + 1) * 64], in0=pov[:, tt * 65:tt * 65 + 64],
                        scalar1=rc[:, tt:tt + 1], scalar2=None, op0=ALU.mult),
                        reads=poid + [("rec", b)], writes=[("atok", ab)])
                if h == H - 1:
                    def cat_tail(i=i, ab=ab):
                        trv, trid = tr3_bank()
                        for tt in range(2):
                            for cc in range(4):
                                P.op("pe", lambda e, tt=tt, cc=cc: e.transpose(
                                    out=trv[:, cc * 256 + tt * 128:cc * 256 + (tt + 1) * 128],
                                    in_=atok[ab][:, tt, cc * 128:(cc + 1) * 128], identity=ident),
                                    reads=[("atok", ab), "ident"], writes=trid)
                        P.op("dve", lambda e: e.tensor_copy(out=CAT[:, 0:4, i * 256:(i + 1) * 256],
                                                            in_=trv.rearrange("p (c s) -> p c s", c=4)),
                             reads=trid, writes=[("cat", c, 2 * i + t_) for c in range(4) for t_ in range(2)])
                    cat_pending.append([6, cat_tail])

        tr3_ctr = [0]
        cat_pending = []

        def cat_tick(force=False):
            for ent in list(cat_pending):
                ent[0] -= 1
                if ent[0] <= 0 or force:
                    cat_pending.remove(ent)
                    ent[1]()

        def tr3_bank():
            n = tr3_ctr[0] % 2
            tr3_ctr[0] += 1
            return psb[6 + n][:, :].bitcast(BF16), fb_(6 + n)

        gate_pending = {}
        gate_chains = {}

        def gate_chain(i, tt):
            gate_chains[i][tt]()

        def gate_front(i):
            tails = []
            gid = fb_(3)
            for tt in range(2):
                t = 2 * i + tt
                gvf = psb[3][:, tt * 128:(tt + 1) * 128]
                for h in range(8):
                    P.op("pe", lambda e, gvf=gvf, h=h, t=t: e.matmul(
                        out=gvf[:, h * 16:(h + 1) * 16], lhsT=QT[0:64, h, t * 128:(t + 1) * 128],
                        rhs=kmT[0:64, h * 16:(h + 1) * 16], start=True, stop=True),
                        reads=[("qT", h, i)] + [("kmT", s_) for s_ in range(16)], writes=gid)
            chains = []
            for tt in range(2):
              def chain(tt=tt):
                t = 2 * i + tt
                gvf = psb[3][:, tt * 128:(tt + 1) * 128]
                gmv = gm.rearrange("p (h s) -> p h s", h=8)
                elb = elig[:, i, :].unsqueeze(1).to_broadcast([128, 8, 16])
                owb = owntab[:, i, :].unsqueeze(1).to_broadcast([128, 8, 16])
                stg = stage[tt]
                P.op("dve", lambda e, gvf=gvf, gmv=gmv, elb=elb: e.tensor_tensor(
                    out=gmv, in0=gvf[:, 0:128].rearrange("p (h s) -> p h s", h=8), in1=elb, op=ALU.add),
                    reads=gid + ["elig"], writes=GMH)
                for h in range(8):
                    P.op("dve", lambda e, h=h: e.max(out=m8[:, h * 8:(h + 1) * 8], in_=gm[:, h * 16:(h + 1) * 16]),
                         reads=[("gmh", h)], writes=[("m8", h)])
                for h in range(8):
                    P.op("dve", lambda e, h=h: e.tensor_scalar(
                        out=gm[:, h * 16:(h + 1) * 16], in0=gm[:, h * 16:(h + 1) * 16],
                        scalar1=m8[:, h * 8 + 2:h * 8 + 3], scalar2=-BIG, op0=ALU.is_lt, op1=ALU.mult),
                        reads=[("gmh", h), ("m8", h)], writes=[("gmh", h)])
                P.op("dve", lambda e, gmv=gmv, elb=elb: e.tensor_tensor(out=gmv, in0=gmv, in1=elb, op=ALU.add),
                     reads=["elig"] + GMH, writes=GMH)
                P.op("dve", lambda e, gmv=gmv, owb=owb, stg=stg: e.tensor_tensor(
                    out=stg.rearrange("p (h s) -> p h s", h=8), in0=gmv, in1=owb, op=ALU.max),
                    reads=GMH + ["owntab"], writes=[("stage", tt)])

                def tail(t=t, tt=tt, stg=stg):
                    trv, trid = tr3_bank()
                    for h in range(8):
                        P.op("pe", lambda e, h=h: e.transpose(out=trv[0:16, h * 128:(h + 1) * 128],
                                                              in_=stg[:, h * 16:(h + 1) * 16], identity=ident),
                             reads=[("stage", tt), "ident"], writes=trid)
                    P.op("dve", lambda e: e.tensor_copy(out=QT[64:80, :, t * 128:(t + 1) * 128],
                                                        in_=trv[0:16, :].rearrange("p (h s) -> p h s", h=8)),
                         reads=trid, writes=[("qM", t)])
                tails.append(tail)
              chains.append(chain)
            gate_pending[i] = tails
            gate_chains[i] = chains

        def gate_tail(i):
            for f_ in gate_pending.pop(i):
                f_()

        gate_front(0)
        gate_chain(0, 0)
        gate_chain(0, 1)
        gate_tail(0)
        pend = []
        prev_ih = None
        for unit in units:
            ui, uh, uu = unit
            if (ui, uh) != prev_ih and ui + 1 < NOWN:
                if uh == 3:
                    gate_front(ui + 1)
                elif uh == 4:
                    gate_chain(ui + 1, 0)
                elif uh == 5:
                    gate_chain(ui + 1, 1)
                elif uh == 7:
                    gate_tail(ui + 1)
            prev_ih = (ui, uh)
            n = emit_qk(unit)
            pend.append((unit, n))
            if len(pend) > 2:
                emit_pv(*pend.pop(0))
            cat_tick()
        while pend:
            emit_pv(*pend.pop(0))
        cat_tick(force=True)

        if debug:
            P.op("sp", lambda e: e.dma_start(out=dbg["qt"][0:84, :], in_=RBC[0:84, 16640:33024]),
                 reads=[("qT", h, i) for h in range(8) for i in range(8)] + [("qM", t) for t in range(NT)] + ["qaug"],
                 dma=True)
            P.op("sp", lambda e: e.dma_start(out=dbg["kt"][0:84, :], in_=RA[0:84, :]), reads=kt_ids, dma=True)
            P.op("sp", lambda e: e.dma_start(out=dbg["vp"], in_=RBC[:, 0:16640]),
                 reads=[("V", c) for c in range(32)] + ["vones"], dma=True)

        if debug:
            P.op("sp", lambda e: e.dma_start(out=dbg["cat"], in_=RD[:, :]),
                 reads=[("cat", c, t) for c in range(8) for t in range(NT)], dma=True)

        p3_old = ([("PT", n) for n in range(4)] + [("atok", n) for n in range(2)] + [("rec", n) for n in range(2)]
                  + [("gmh", h) for h in range(8)] + [("m8", h) for h in range(8)] + [("stage", t_) for t_ in range(2)]
                  + [("kmT", ks) for ks in range(16)] + ["elig", "owntab"]
                  + kt_ids + [("V", c) for c in range(32)] + ["vones", "qaug"]
                  + [("qT", h, i) for h in range(8) for i in range(8)] + [("qM", t) for t in range(NT)])
        cv.off = persist_end
        gb = cv.f32(1024)
        hb4 = [cv.bf(1024), cv.bf(1024), cv.bf(1024)]
        actT = [cv.bf(2048).rearrange("p (f s) -> p f s", f=8) for _ in range(2)]
        rtmp = [cv.f32(256) for _ in range(2)]
        xr = [cv.f32(1024), cv.f32(1024)]
        p4_ids = (["gb", ("hb4", 0), ("hb4", 1), ("hb4", 2)] + [("actT", n, fb) for n in range(2) for fb in range(8)] + [("rtmp", n) for n in range(2)]
                  + [("xr", n) for n in range(2)] + [("xm", t, c) for t in range(NT) for c in range(2)]
                  + [("wup", s, kc) for s in range(2) for kc in range(8)]
                  + [("wdn", s, kc) for s in range(2) for kc in range(8)])
        fence(p3_old, p4_ids)

        def load_ffn(fg):
            s = fg % 2
            wu, wd = ring(s)
            for kc in range(8):
                P.op("pool", lambda e, kc=kc: e.dma_start(
                    out=wu[:, kc, :], in_=w_up[kc * 128:(kc + 1) * 128, fg * 1024:(fg + 1) * 1024]),
                    writes=[("wup", s, kc)], dma=True)
            for fb in range(8):
                P.op("pool", lambda e, fb=fb: e.dma_start(
                    out=wd[:, fb, :], in_=w_down[fg * 1024 + fb * 128:fg * 1024 + (fb + 1) * 128, :]),
                    writes=[("wdn", s, fb)], dma=True)

        load_ffn(0)
        load_ffn(1)
        P.op("sp", lambda e: e.dma_start(out=gb, in_=g_mlp.partition_broadcast(128)), writes=["gb"], dma=True)

        tr4_ctr = [0]

        def wout_mm(t):
            slot = t % 2
            P.op("sp", lambda e: e.dma_start(out=xr[slot], in_=x_own[t * 128:(t + 1) * 128, :]),
                 writes=[("xr", slot)], dma=True)
            n = t % 3
            for ch in range(2):
                bank = psb[2 * n + ch]
                bid = fb_(2 * n + ch)
                for kc in range(8):
                    P.op("pe", lambda e, bank=bank, kc=kc, ch=ch: e.matmul(
                        out=bank[:, :], lhsT=CAT[:, kc, t * 128:(t + 1) * 128], rhs=WOUT[:, kc, ch * 512:(ch + 1) * 512],
                        start=(kc == 0), stop=(kc == 7)),
                        reads=[("cat", kc, t), ("wout", kc)], writes=bid)
                P.op("dve", lambda e, bank=bank, ch=ch: e.tensor_tensor(
                    out=XM[:, t, ch * 512:(ch + 1) * 512], in0=bank[:, :], in1=xr[slot][:, ch * 512:(ch + 1) * 512], op=ALU.add),
                    reads=bid + [("xr", slot)], writes=[("xm", t, ch)])
            col = 34 + t
            norm_rs(col, XM[:, t, :], [("xm", t, 0), ("xm", t, 1)], hb4[t % 3], ("hb4", t % 3))
            P.op("dve", lambda e: e.scalar_tensor_tensor(out=hb4[t % 3], in0=XM[:, t, :], scalar=rs[:, col:col + 1], in1=gb,
                                                         op0=ALU.mult, op1=ALU.mult),
                 reads=[("xm", t, 0), ("xm", t, 1), ("rs", col), "gb"], writes=[("hb4", t % 3)])

        def wout_tail(t):
            k = tr4_ctr[0] % 2
            tr4_ctr[0] += 1
            trv = psb[6 + k][:, :].bitcast(BF16)
            trid = fb_(6 + k)
            for kc in range(8):
                P.op("pe", lambda e, kc=kc: e.transpose(out=trv[:, kc * 128:(kc + 1) * 128],
                                                        in_=hb4[t % 3][:, kc * 128:(kc + 1) * 128], identity=ident),
                     reads=[("hb4", t % 3), "ident"], writes=trid)
            P.op("act", lambda e: e.copy(out=CAT[:, :, t * 128:(t + 1) * 128], in_=trv.rearrange("p (k s) -> p k s", k=8)),
                 reads=trid, writes=[("cat", c, t) for c in range(8)])

        for t in range(NT):
            wout_mm(t)
            if t >= 2:
                wout_tail(t - 2)
        ffn_inject = {0: [lambda: wout_tail(NT - 2)], 1: [lambda: wout_tail(NT - 1)]}

        pu_ctr = [0]
        pd_ctr = [0]
        act_ctr = [0]
        rt_ctr = [0]

        def ffn_up(fg, tp):
            s = fg % 2
            wu, _ = ring(s)
            ab = act_ctr[0] % 2
            act_ctr[0] += 1
            for fb in range(8):
                n = pu_ctr[0] % 4
                pu_ctr[0] += 1
                pv = psb[n][:, 0:256]
                pid = ("ps", n)
                for kc in range(8):
                    P.op("pe", lambda e, pv=pv, kc=kc, fb=fb: e.matmul(
                        out=pv, lhsT=wu[:, kc, fb * 128:(fb + 1) * 128], rhs=CAT[:, kc, tp * 256:(tp + 1) * 256],
                        start=(kc == 0), stop=(kc == 7)),
                        reads=[("wup", s, kc), ("cat", kc, 2 * tp), ("cat", kc, 2 * tp + 1)], writes=[pid])
                r = rt_ctr[0] % 2
                rt_ctr[0] += 1
                P.op("act", lambda e, pv=pv, r=r: e.activation(out=rtmp[r], in_=pv, func=AF.Relu),
                     reads=[pid], writes=[("rtmp", r)])
                P.op("dve", lambda e, r=r, fb=fb, ab=ab: e.tensor_tensor(out=actT[ab][:, fb, :], in0=rtmp[r], in1=rtmp[r],
                                                                       op=ALU.mult),
                     reads=[("rtmp", r)], writes=[("actT", ab, fb)])
            return ab

        def ffn_down(fg, tp, ab):
            s = fg % 2
            _, wd = ring(s)
            for tt in range(2):
                t = 2 * tp + tt
                for ch in range(2):
                    n = pd_ctr[0] % 4
                    pd_ctr[0] += 1
                    bank = psb[4 + n]
                    bid = fb_(4 + n)
                    for fb in range(8):
                        P.op("pe", lambda e, bank=bank, fb=fb, tt=tt, ch=ch: e.matmul(
                            out=bank[:, :], lhsT=actT[ab][:, fb, tt * 128:(tt + 1) * 128],
                            rhs=wd[:, fb, ch * 512:(ch + 1) * 512], start=(fb == 0), stop=(fb == 7)),
                            reads=[("actT", ab, fb), ("wdn", s, fb)], writes=bid)
                    P.op("dve", lambda e, bank=bank, t=t, ch=ch: e.tensor_tensor(
                        out=XM[:, t, ch * 512:(ch + 1) * 512], in0=bank[:, :], in1=XM[:, t, ch * 512:(ch + 1) * 512],
                        op=ALU.add),
                        reads=bid + [("xm", t, ch)], writes=[("xm", t, ch)])

        out_ops = []

        def final_tile(t):
            col = 50 + t
            norm_rs(col, XM[:, t, :], [("xm", t, 0), ("xm", t, 1)], hb4[t % 3], ("hb4", t % 3))
            slot = t % 2
            P.op("dve", lambda e: e.scalar_tensor_tensor(out=xr[slot], in0=XM[:, t, :], scalar=rs[:, col:col + 1], in1=gb,
                                                         op0=ALU.mult, op1=ALU.mult),
                 reads=[("xm", t, 0), ("xm", t, 1), ("rs", col), "gb"], writes=[("xr", slot)])
            out_ops.append(P.op("sp", lambda e: e.dma_start(out=y[t * 128:(t + 1) * 128, :], in_=xr[slot]),
                                reads=[("xr", slot)], dma=True))

        seq = [(fg, tp) for fg in range(4) for tp in range(8)]
        prev = None
        loaded = 2
        gfin_loaded = False
        for idx, (fg, tp) in enumerate(seq):
            ab = ffn_up(fg, tp)
            for f_ in ffn_inject.pop(idx, []):
                f_()
            if prev is not None:
                pfg, ptp, pab = prev
                ffn_down(pfg, ptp, pab)
                if ptp == 7 and pfg + 2 < 4:
                    load_ffn(pfg + 2)
                if pfg == 3:
                    if not gfin_loaded:
                        P.op("sp", lambda e: e.dma_start(out=gb, in_=g_fin.partition_broadcast(128)), writes=["gb"], dma=True)
                        gfin_loaded = True
                    final_tile(2 * ptp)
                    final_tile(2 * ptp + 1)
            prev = (fg, tp, ab)
        pfg, ptp, pab = prev
        ffn_down(pfg, ptp, pab)
        final_tile(2 * ptp)
        final_tile(2 * ptp + 1)

        dbg_ops = [o for o in P.ops if o.dma and o.eng == "sp" and not o.writes and o not in out_ops]
        P.op("sp", lambda e: None, after=out_ops + dbg_ops)
        stats = P.emit(st)
    return nc, stats


def _core_tables(r):
    bf = ml_dtypes.bfloat16
    own_g = [2 * i + r for i in range(NOWN)]
    ctx_g = [(2 * i - 1 + r) % NBLK for i in range(NOWN)]
    slot_g = []
    for i in range(NOWN):
        slot_g += [ctx_g[i], own_g[i]]
    kaug = np.zeros((20, 4096), np.float32)
    for ks in range(16):
        sl = slice(ks * 256, (ks + 1) * 256)
        kaug[ks, sl] = 1.0
        kaug[16, sl] = 1.0
        kaug[17, sl] = 1.0
        kaug[18, sl] = np.arange(256)
        kaug[19, sl] = slot_g[ks]
    qaug = np.zeros((4, H, NTOK), np.float32)
    for h in range(H):
        s = SLOPES[h]
        for i in range(NOWN):
            sl = slice(i * 256, (i + 1) * 256)
            qaug[0, h, sl] = -s * np.arange(256)
            qaug[1, h, sl] = -s * 256.0 * own_g[i]
            qaug[2, h, sl] = s
            qaug[3, h, sl] = s * 256.0
    elig = np.zeros((128, NOWN, 16), np.float32)
    owntab = np.full((128, NOWN, 16), -3.0 * BIG, np.float32)
    for i in range(NOWN):
        for ks in range(16):
            elig[:, i, ks] = 0.0 if slot_g[ks] < own_g[i] else -BIG
        owntab[:, i, 2 * i + 1] = 0.0
    tri = np.where(np.arange(128)[:, None] <= np.arange(128)[None, :], 0.0, -BIG).astype(np.float32)
    ident = np.eye(128, dtype=np.float32)
    cntfix = np.zeros((128, 4, 16), np.float32)
    for g, w in enumerate(WINS):
        pos = own_g[0] * 256 + np.arange(16)
        cntfix[:, g, :] = float(w) / np.minimum(pos + 1, w)
    return dict(kaug=kaug.astype(bf), qaug=qaug.reshape(4, H * NTOK).astype(bf),
                elig=elig.reshape(128, -1).astype(bf), owntab=owntab.reshape(128, -1).astype(bf),
                tri=tri.astype(bf), ident=ident.astype(bf), cntfix=cntfix.reshape(128, 64)), own_g, ctx_g


_CACHE = {}


def kernel(x, norm_mix, w_in, w_pool, pool_scale, w_out, norm_mlp, w_up, w_down, norm_final, _debug=False):
    x = np.ascontiguousarray(np.asarray(x, dtype=np.float32))
    f = lambda a: np.ascontiguousarray(np.asarray(a, dtype=np.float32))
    key = ("nc", bool(_debug))
    if key not in _CACHE:
        _CACHE[key] = build_program(debug=_debug)
    nc, stats = _CACHE[key]
    shared = dict(
        w_in=f(w_in)[0], w_out=f(w_out)[0], w_up=f(w_up)[0], w_down=f(w_down)[0],
        w_pool=f(w_pool)[0].reshape(512, 128),
        pscale_t=np.ascontiguousarray(f(pool_scale)[0].reshape(4, 128).T),
        gmix_t=np.ascontiguousarray(f(norm_mix)[0].reshape(8, 128).T),
        g_mlp=f(norm_mlp)[0], g_fin=f(norm_final),
    )
    in_maps = []
    meta = []
    for c in range(NCORES):
        b, r = divmod(c, 2)
        tabs, own_g, ctx_g = _core_tables(r)
        xb = x[b].reshape(NBLK, BLK, D)
        x_own = np.ascontiguousarray(xb[own_g].reshape(NTOK, D))
        x_ctx = np.ascontiguousarray(xb[ctx_g].reshape(NTOK, D))
        x_hist = np.zeros((NOWN, 16, D), np.float32)
        for i, g in enumerate(own_g):
            if g > 0:
                x_hist[i] = x[b, g * BLK - 16:g * BLK]
        m = dict(shared)
        m.update(tabs)
        m.update(x_own=x_own, x_ctx=x_ctx, x_hist=x_hist.reshape(128, D))
        in_maps.append(m)
        meta.append((b, own_g))
    res = run_bass_kernel_spmd(nc, in_maps, core_ids=list(range(NCORES)))
    out = np.empty((NBATCH, SEQ, D), np.float32)
    for c in range(NCORES):
        b, own_g = meta[c]
        yb = np.asarray(res.results[c]["y"]).reshape(NOWN, BLK, D)
        ob = out[b].reshape(NBLK, BLK, D)
        for i, g in enumerate(own_g):
            ob[g] = yb[i]
    if _debug:
        return out, res
    return out
```

```python
import numpy as np
import ml_dtypes
from contextlib import ExitStack
import concourse.bass as bass
import concourse.mybir as mybir
from concourse.bass_utils import run_bass_kernel_spmd

F32 = mybir.dt.float32
BF16 = mybir.dt.bfloat16
ALU = mybir.AluOpType
AF = mybir.ActivationFunctionType

ENGS = ("pe", "act", "dve", "pool", "sp")


class Op:
    __slots__ = ("eng", "fn", "reads", "writes", "dma", "idx", "deps", "waits",
                 "signal", "dom", "val", "clock", "extra")

    def __init__(self, eng, fn, reads, writes, dma):
        self.eng = eng
        self.fn = fn
        self.reads = tuple(reads)
        self.writes = tuple(writes)
        self.dma = dma
        self.signal = False
        self.waits = []
        self.deps = []
        self.extra = []
        self.idx = 0
        self.val = 0


class Prog:
    def __init__(self, nc, n_dma_sems=20):
        self.nc = nc
        self.ops = []
        self.n_dma_sems = n_dma_sems

    def op(self, eng, fn, reads=(), writes=(), dma=False, after=()):
        o = Op(eng, fn, reads, writes, dma)
        o.extra = list(after)
        self.ops.append(o)
        return o

    def _analyze(self):
        last_write = {}
        reads_since = {}
        eng_count = {e: 0 for e in ENGS}
        dma_rr = {e: 0 for e in ENGS}
        dma_last = {}
        dma_val = {}
        for o in self.ops:
            if o.dma:
                k = dma_rr[o.eng] % self.n_dma_sems
                dma_rr[o.eng] += 1
                o.dom = ("dma", o.eng, k)
                o.val = dma_val.get(o.dom, 0) + 16
                dma_val[o.dom] = o.val
                prev = dma_last.get(o.dom)
                if prev is not None:
                    o.deps.append(prev)
                dma_last[o.dom] = o
            else:
                o.dom = o.eng
                eng_count[o.eng] += 1
                o.idx = eng_count[o.eng]
            deps = list(o.extra)
            for b in o.reads:
                lw = last_write.get(b)
                if lw:
                    deps.extend(lw.values())
            for b in o.writes:
                lw = last_write.get(b)
                if lw:
                    deps.extend(lw.values())
                rs = reads_since.get(b)
                if rs:
                    deps.extend(rs.values())
            seen = set()
            for d in deps:
                if d is o or id(d) in seen:
                    continue
                seen.add(id(d))
                if (not d.dma) and (not o.dma) and d.eng == o.eng:
                    if o.eng == "pe":
                        continue
                o.deps.append(d)
            for b in o.reads:
                reads_since.setdefault(b, {})[o.dom] = o
            for b in o.writes:
                last_write[b] = {o.dom: o}
                reads_since[b] = {}
        know = {e: {} for e in ENGS}
        for o in self.ops:
            kn = know[o.eng]
            for d in o.deps:
                dv = d.val if d.dma else d.idx
                if (not d.dma) and d.eng == o.eng:
                    key = ("self", d.dom)
                    if kn.get(key, 0) >= dv:
                        continue
                    kn[key] = dv
                    o.waits.append(d)
                    d.signal = True
                    continue
                if kn.get(d.dom, 0) >= dv:
                    continue
                o.waits.append(d)
                d.signal = True
                kn[d.dom] = dv
                for k2, v2 in d.clock.items():
                    if isinstance(k2, tuple) and k2[0] == "self":
                        continue
                    if kn.get(k2, 0) < v2:
                        kn[k2] = v2
            o.clock = dict(kn)
            if not o.dma:
                o.clock[o.dom] = o.idx
        cnt = {e: 0 for e in ENGS}
        for o in self.ops:
            if o.dma:
                o.signal = True
                continue
            if o.signal:
                cnt[o.eng] += 1
                o.val = cnt[o.eng]

    def emit(self, stack):
        nc = self.nc
        self._analyze()
        sems = {}
        for e in ENGS:
            sems[e] = stack.enter_context(nc.semaphore("s_" + e))
        for o in self.ops:
            if o.dma and o.dom not in sems:
                sems[o.dom] = stack.enter_context(
                    nc.semaphore("d_%s_%d" % (o.dom[1], o.dom[2])))
        block = stack.enter_context(nc.Block())
        by_eng = {e: [o for o in self.ops if o.eng == e] for e in ENGS}

        def run(eng_name):
            def body(eng):
                for o in by_eng[eng_name]:
                    for d in o.waits:
                        eng.wait_ge(sems[d.dom], d.val)
                    ins = o.fn(eng)
                    if o.signal and ins is not None:
                        ins.then_inc(sems[o.dom], 16 if o.dma else 1)
            return body

        block.tensor(run("pe"))
        block.scalar(run("act"))
        block.vector(run("dve"))
        block.gpsimd(run("pool"))
        block.sync(run("sp"))
        return dict(n_ops=len(self.ops), n_waits=sum(len(o.waits) for o in self.ops),
                    per_eng={e: len(by_eng[e]) for e in ENGS})


D = 1024
SEQ = 4096
NBATCH = 4
H = 8
DH = 64
BLK = 256
NBLK = 16
NOWN = 8
NTOK = NOWN * BLK
NT = NTOK // 128
DFF = 4096
EPS = 1e-6
BIG = 30000.0
ALIBI_CUT = 45.0
WINS = (2, 4, 8, 16)
SLOPES = [2.0 ** (-(h + 1)) for h in range(H)]
NCORES = 8


def build_program(debug=False):
    nc = bass.Bass("TRN2", target_bir_lowering=False)

    def din(name, shape, dt=F32):
        return nc.dram_tensor(name, shape, dt, kind="ExternalInput").ap()

    x_own = din("x_own", [NTOK, D])
    x_ctx = din("x_ctx", [NTOK, D])
    x_hist = din("x_hist", [128, D])
    w_in = din("w_in", [D, 2048])
    w_out = din("w_out", [D, D])
    w_up = din("w_up", [D, DFF])
    w_down = din("w_down", [DFF, D])
    w_pool = din("w_pool", [512, 128])
    pscale_t = din("pscale_t", [128, 4])
    gmix_t = din("gmix_t", [128, 8])
    g_mlp = din("g_mlp", [D])
    g_fin = din("g_fin", [D])
    kaug = din("kaug", [20, 4096], BF16)
    qaug = din("qaug", [4, H * NTOK], BF16)
    elig_d = din("elig", [128, NOWN * 16], BF16)
    owntab_d = din("owntab", [128, NOWN * 16], BF16)
    tri_d = din("tri", [128, 128], BF16)
    ident_d = din("ident", [128, 128], BF16)
    cntfix_d = din("cntfix", [128, 64])
    y = nc.dram_tensor("y", [NTOK, D], F32, kind="ExternalOutput").ap()
    dbg = {}
    if debug:
        dbg["cat"] = nc.dram_tensor("dbg_cat", [128, 8 * NTOK], BF16, kind="ExternalOutput").ap()
        dbg["qt"] = nc.dram_tensor("dbg_qt", [128, 8 * NTOK], BF16, kind="ExternalOutput").ap()
        dbg["kt"] = nc.dram_tensor("dbg_kt", [128, 8 * 4096], BF16, kind="ExternalOutput").ap()
        dbg["vp"] = nc.dram_tensor("dbg_vp", [128, 32 * 8 * 65], BF16, kind="ExternalOutput").ap()

    with ExitStack() as st:
        RA = st.enter_context(nc.sbuf_tensor("RA", [128, 32768], BF16))
        RBC = st.enter_context(nc.sbuf_tensor("RBC", [128, 33024], BF16))
        RD = st.enter_context(nc.sbuf_tensor("RD", [128, 16384], BF16))
        RE = st.enter_context(nc.sbuf_tensor("RE", [128, 8192], BF16))
        NW = 16032
        RW = st.enter_context(nc.sbuf_tensor("RW", [128, NW], BF16))
        psb = [st.enter_context(nc.psum_tensor("ps%d" % b, [128, 512], F32)) for b in range(8)]

        KT = RA[:, :].rearrange("p (h s) -> p h s", h=8)
        WST = RA[:, :].bitcast(F32).rearrange("p (k n) -> p k n", k=8)
        XM = RA[:, :].bitcast(F32).rearrange("p (t d) -> p t d", t=16)
        VP = RBC[:, 0:16640].rearrange("p (c h e) -> p c h e", c=32, h=8)
        VP1 = RBC[:, 0:16640].rearrange("p (m e) -> p m e", e=65)
        QT = RBC[:, 16640:33024].rearrange("p (h s) -> p h s", h=8)
        CAT = RD[:, :].rearrange("p (c s) -> p c s", c=8)
        WQK = RD[:, 0:8192].rearrange("p (k n) -> p k n", k=8)
        WVU = RE[:, :].rearrange("p (k n) -> p k n", k=8)
        WOUT = WVU

        def ring(slot):
            base = slot * 16384
            wu = RBC[:, base:base + 8192].rearrange("p (k n) -> p k n", k=8)
            wd = RBC[:, base + 8192:base + 16384].rearrange("p (k n) -> p k n", k=8)
            return wu, wd

        class Carver:
            def __init__(self):
                self.off = 0

            def bf(self, n):
                a = RW[:, self.off:self.off + n]
                self.off += n
                assert self.off <= NW, self.off
                return a

            def f32(self, n):
                if self.off % 2:
                    self.off += 1
                a = RW[:, self.off:self.off + 2 * n].bitcast(F32)
                self.off += 2 * n
                assert self.off <= NW, self.off
                return a

        cv = Carver()
        tri = cv.bf(128)
        ident = cv.bf(128)
        ss = cv.f32(68)
        rs = cv.f32(68)
        dummy = cv.f32(16)
        epsb = cv.f32(2)
        elig = cv.bf(128).rearrange("p (i s) -> p i s", i=8)
        owntab = cv.bf(128).rearrange("p (i s) -> p i s", i=8)
        kmT = cv.bf(128)
        persist_end = cv.off

        cntfix = cv.f32(64)
        wpool = cv.bf(512).rearrange("p (g d) -> p g d", g=4)
        pscale = cv.f32(4)
        cv_pscw = cv.f32(4)
        gcol = cv.f32(8)
        ksum = cv.f32(128)
        fx = cv.f32(16)
        uH = cv.f32(512).rearrange("p (g s) -> p g s", g=4)
        ub = [cv.f32(272), cv.f32(272)]
        sS = [cv.f32(272) for _ in range(3)]
        mixT = [cv.bf(256) for _ in range(4)]
        hb = cv.bf(1024)
        hTb = [cv.bf(2048).rearrange("p (k s) -> p k s", k=8) for _ in range(2)]
        xt = [cv.f32(1024), cv.f32(1024)]
        p1_end = cv.off

        P = Prog(nc)

        def fb_(bank):
            return [("ps", bank)]
        dummy_i = [0]

        def fence(old_ids, new_ids):
            j = dummy_i[0] % 16
            dummy_i[0] += 1
            return P.op("pool", lambda e: e.memset(dummy[0:1, j:j + 1], 0.0),
                        writes=list(old_ids) + list(new_ids) + [("fx_dummy", j)])

        P.op("sp", lambda e: e.dma_start(out=tri, in_=tri_d), writes=["tri"], dma=True)
        P.op("sp", lambda e: e.dma_start(out=ident, in_=ident_d), writes=["ident"], dma=True)
        P.op("sp", lambda e: e.dma_start(out=gcol, in_=gmix_t), writes=["gcol"], dma=True)
        w_in_v = w_in.rearrange("(k p) n -> p k n", p=128)
        WGRP = [("wu", 1536, WVU, 512), ("wk", 512, WQK, 512), ("wv", 1024, WVU, 0), ("wq", 0, WQK, 0)]
        P.op("sp", lambda e: e.dma_start(out=cntfix, in_=cntfix_d), writes=["cntfix"], dma=True)
        P.op("sp", lambda e: e.dma_start(out=elig.rearrange("p i s -> p (i s)"), in_=elig_d), writes=["elig"], dma=True)
        P.op("sp", lambda e: e.dma_start(out=owntab.rearrange("p i s -> p (i s)"), in_=owntab_d), writes=["owntab"], dma=True)
        P.op("sp", lambda e: e.dma_start(out=pscale, in_=pscale_t), writes=["pscale"], dma=True)
        P.op("pool", lambda e: e.dma_start(out=wpool, in_=w_pool.rearrange("(g c) d -> c g d", g=4)),
             writes=["wpool"], dma=True)
        P.op("sp", lambda e: e.dma_start(out=QT[80:84, :, :], in_=qaug.rearrange("r (h s) -> r h s", h=8)),
             writes=["qaug"], dma=True)
        P.op("pool", lambda e: e.memset(VP1[:, :, 64:65], 1.0), writes=["vones"])
        P.op("pool", lambda e: e.memset(ksum, 0.0), writes=[("ksum", h, ks) for h in range(8) for ks in range(16)])
        P.op("pool", lambda e: e.memset(kmT, 0.0), writes=[("kmT", ks) for ks in range(16)])
        P.op("pool", lambda e: e.memset(ss, 0.0), writes=[("ss", c) for c in range(68)])
        P.op("pool", lambda e: e.memset(epsb, EPS), writes=["epsb"])

        pscw = cv_pscw
        for g in range(4):
            P.op("dve", lambda e, g=g: e.tensor_scalar(out=pscw[:, g:g + 1], in0=pscale[:, g:g + 1],
                                                      scalar1=1.0 / WINS[g], scalar2=None, op0=ALU.mult),
                 reads=["pscale"], writes=["pscw"])
        kt_ids = [("kT", h, ks) for h in range(8) for ks in range(16)] + [("kaug", h) for h in range(8)]
        tile_ctr = [0]
        blk_ctr = [0]
        pp_ctr = [0]
        pv_ctr = [0]
        tr_ctr = [0]
        ub_ctr = [0]
        mix_ctr = [0]

        def pp_slot():
            n = pp_ctr[0] % 4
            pp_ctr[0] += 1
            bank = 2 + n
            return psb[bank][:, 0:256], ("ps", bank)

        def tr_bank():
            n = tr_ctr[0] % 2
            tr_ctr[0] += 1
            return psb[n][:, :].bitcast(BF16), fb_(n)

        def norm_rs(col, src_ap, src_ids, junk, junk_id):
            P.op("act", lambda e: e.activation(out=junk, in_=src_ap, func=AF.Square, accum_out=ss[:, col:col + 1]),
                 reads=src_ids, writes=[junk_id, ("ss", col)])
            P.op("act", lambda e: e.activation(out=rs[:, col:col + 1], in_=ss[:, col:col + 1], func=AF.Ln,
                                               scale=1.0 / D, bias=epsb[:, 0:1]),
                 reads=[("ss", col), "epsb"], writes=[("rs", col)])
            P.op("act", lambda e: e.activation(out=rs[:, col:col + 1], in_=rs[:, col:col + 1], func=AF.Exp, scale=-0.5),
                 reads=[("rs", col)], writes=[("rs", col)])

        def a_begin(kind, i):
            bidx = blk_ctr[0]
            blk_ctr[0] += 1
            return bidx % 2

        def a_tile(kind, i, buf, tt, split=False):
            if kind == "hist" and tt == 1:
                return
            if True:
                tc_ = tile_ctr[0]
                tile_ctr[0] += 1
                slot = tc_ % 2
                col = tc_
                if kind == "hist":
                    src = x_hist
                elif kind == "ctx":
                    src = x_ctx[(i * 2 + tt) * 128:(i * 2 + tt + 1) * 128, :]
                else:
                    src = x_own[(i * 2 + tt) * 128:(i * 2 + tt + 1) * 128, :]
                xts = xt[slot]
                P.op("sp", lambda e, xts=xts, src=src: e.dma_start(out=xts, in_=src), writes=[("xt", slot)], dma=True)
                norm_rs(col, xts, [("xt", slot)], hb, "hb")
                P.op("act", lambda e, xts=xts, col=col: e.activation(out=hb, in_=xts, func=AF.Copy,
                                                                    scale=rs[:, col:col + 1]),
                     reads=[("xt", slot), ("rs", col)], writes=["hb"])

                def a_trans(buf=buf, tt=tt):
                    trv, trid = tr_bank()
                    for kc in range(8):
                        P.op("pe", lambda e, trv=trv, kc=kc: e.transpose(out=trv[:, kc * 128:(kc + 1) * 128],
                                                                         in_=hb[:, kc * 128:(kc + 1) * 128], identity=ident),
                             reads=["hb", "ident"], writes=trid)
                    P.op("dve", lambda e, trv=trv: e.tensor_copy(
                        out=hTb[buf][:, :, tt * 128:(tt + 1) * 128], in_=trv.rearrange("p (k s) -> p k s", k=8)),
                        reads=trid, writes=[("hT", buf, tt)])
                if split:
                    return a_trans
                a_trans()

        def proj(buf, wview, wname, c0, ntok):
            pv, pid = pp_slot()
            for kc in range(8):
                P.op("pe", lambda e, pv=pv, kc=kc: e.matmul(out=pv[:, 0:ntok], lhsT=wview[:, kc, c0:c0 + 128],
                                                            rhs=hTb[buf][:, kc, 0:ntok], start=(kc == 0), stop=(kc == 7)),
                     reads=[(wname, kc), ("hT", buf, 0), ("hT", buf, 1)], writes=[pid])
            return pv, pid

        deferred = []
        GMH = [("gmh", h) for h in range(8)]

        def flush_deferred():
            while deferred:
                deferred.pop(0)()

        gps_state = {}

        def stage_b1(kind, i, buf):
            if kind == "hist":
                for g in range(4):
                    pv, pid = proj(buf, WVU, "wu", 512 + g * 128, 128)
                    P.op("dve", lambda e, pv=pv, g=g: e.tensor_copy(out=uH[:, g, :], in_=pv[:, 0:128]),
                         reads=[pid], writes=[("uH", g)])
                return
            ks = 2 * i if kind == "ctx" else 2 * i + 1
            for cg in range(4):
                pv, pid = proj(buf, WQK, "wk", 512 + cg * 128, 256)
                for half in range(2):
                    h = 2 * cg + half
                    if half == 0:
                        P.op("act", lambda e, pv=pv, h=h, half=half: e.activation(
                            out=KT[0:64, h, ks * 256:(ks + 1) * 256], in_=pv[half * 64:(half + 1) * 64, 0:256],
                            func=AF.Copy, accum_out=ksum[0:64, h * 16 + ks:h * 16 + ks + 1]),
                            reads=[pid], writes=[("kT", h, ks), ("ksum", h, ks)])
                    else:
                        P.op("dve", lambda e, pv=pv, h=h, half=half: e.tensor_scalar(
                            out=KT[0:64, h, ks * 256:(ks + 1) * 256], in0=pv[half * 64:(half + 1) * 64, 0:256],
                            scalar1=1.0, scalar2=0.0, op0=ALU.mult, op1=ALU.add,
                            accum_out=ksum[0:64, h * 16 + ks:h * 16 + ks + 1]),
                            reads=[pid], writes=[("kT", h, ks), ("ksum", h, ks)])
            ksv = ksum.rearrange("p (h s) -> p h s", h=8)
            kmv = kmT.rearrange("p (h s) -> p h s", h=8)
            P.op("dve", lambda e: e.tensor_scalar(out=kmv[0:64, :, ks:ks + 1], in0=ksv[0:64, :, ks:ks + 1],
                                                  scalar1=1.0 / BLK, scalar2=None, op0=ALU.mult),
                 reads=[("ksum", h, ks) for h in range(8)], writes=[("kmT", ks)])

        def stage_b1v(kind, i, buf):
            if kind == "hist":
                return
            ks = 2 * i if kind == "ctx" else 2 * i + 1
            for tt in range(2):
                n = pv_ctr[0] % 2
                pv_ctr[0] += 1
                pvv = psb[6 + n]
                pvid = fb_(6 + n)
                for kc in range(8):
                    P.op("pe", lambda e, pvv=pvv, kc=kc, tt=tt: e.matmul(
                        out=pvv[:, :], lhsT=hTb[buf][:, kc, tt * 128:(tt + 1) * 128], rhs=WVU[:, kc, 0:512],
                        start=(kc == 0), stop=(kc == 7)),
                        reads=[("wv", kc), ("hT", buf, tt)], writes=pvid)
                chunk = ks * 2 + tt
                P.op("dve", lambda e, pvv=pvv, chunk=chunk: e.tensor_copy(
                    out=VP[:, chunk, :, 0:64], in_=pvv[:, :].rearrange("p (h d) -> p h d", h=8)),
                    reads=pvid, writes=[("V", chunk)])
            flush_deferred()

        def stage_b2(kind, i, buf):
            if kind != "own":
                return
            for cg in range(4):
                pv, pid = proj(buf, WQK, "wq", cg * 128, 256)
                for half in range(2):
                    h = 2 * cg + half
                    P.op("dve", lambda e, pv=pv, h=h, half=half: e.tensor_scalar(
                        out=QT[0:64, h, i * 256:(i + 1) * 256], in0=pv[half * 64:(half + 1) * 64, 0:256],
                        scalar1=DH ** -0.5, scalar2=None, op0=ALU.mult),
                        reads=[pid], writes=[("qT", h, i)])
            combines = []
            for g in range(4):
                pv, pid = proj(buf, WVU, "wu", 512 + g * 128, 256)
                us = ub_ctr[0] % 2
                ub_ctr[0] += 1
                u_ = ub[us]
                uid = ("ub", us)
                P.op("dve", lambda e, pv=pv, u_=u_: e.tensor_copy(out=u_[:, 16:272], in_=pv[:, 0:256]),
                     reads=[pid], writes=[uid])
                P.op("pool", lambda e, u_=u_, g=g: e.tensor_copy(out=u_[:, 0:16], in_=uH[:, g, i * 16:(i + 1) * 16]),
                     reads=[("uH", g)], writes=[(uid, "h")])
                rd = [uid, (uid, "h")]
                if combines:
                    combines.pop(0)()
                sbuf = [sS[g % 3], sS[(g + 1) % 3]]
                sids = [("sS", g % 3), ("sS", (g + 1) % 3)]
                P.op("pool", lambda e, u_=u_, o_=sbuf[0]: e.tensor_tensor(out=o_[:, 1:272], in0=u_[:, 1:272], in1=u_[:, 0:271], op=ALU.add),
                     reads=rd, writes=[sids[0]])
                cur = 0
                lo = 1
                for lvl in range(g):
                    sh = 2 << lvl
                    nlo = lo + sh
                    src_, dst_ = sbuf[cur], sbuf[1 - cur]
                    P.op("pool", lambda e, src_=src_, dst_=dst_, nlo=nlo, sh=sh: e.tensor_tensor(
                        out=dst_[:, nlo:272], in0=src_[:, nlo:272], in1=src_[:, nlo - sh:272 - sh], op=ALU.add),
                        reads=[sids[cur]], writes=[sids[1 - cur]])
                    cur = 1 - cur
                    lo = nlo
                S = sbuf[cur]
                sid = sids[cur]
                mx = mixT[g]
                mid = ("mix", g)

                def combine(g=g, S=S, sid=sid, u_=u_, rd=rd, mx=mx, mid=mid):
                    P.op("dve", lambda e: e.scalar_tensor_tensor(
                        out=mx, in0=u_[:, 16:272], scalar=-float(WINS[g]), in1=S[:, 16:272], op0=ALU.mult, op1=ALU.add),
                        reads=[sid] + rd, writes=[mid])
                    if i == 0:
                        P.op("dve", lambda e: e.tensor_tensor(out=fx[:, 0:16], in0=S[:, 16:32],
                                                              in1=cntfix[:, g * 16:(g + 1) * 16], op=ALU.mult),
                             reads=[sid, "cntfix"], writes=["fx"])
                        P.op("dve", lambda e: e.scalar_tensor_tensor(
                            out=mx[:, 0:16], in0=u_[:, 16:32], scalar=-float(WINS[g]), in1=fx[:, 0:16],
                            op0=ALU.mult, op1=ALU.add),
                            reads=["fx", mid] + rd, writes=[mid])
                combines.append(combine)

                def pool_tail(g=g, mx=mx, mid=mid):
                    pv2, pid2 = pp_slot()
                    P.op("pe", lambda e: e.matmul(out=pv2[:, 0:256], lhsT=wpool[:, g, :], rhs=mx, start=True, stop=True),
                         reads=["wpool", mid], writes=[pid2])
                    P.op("dve", lambda e: e.tensor_scalar(out=CAT[:, 4 + g, i * 256:(i + 1) * 256], in0=pv2[:, 0:256],
                                                          scalar1=pscw[:, g:g + 1], scalar2=None, op0=ALU.mult),
                         reads=[pid2, "pscw"], writes=[("cat", 4 + g, 2 * i), ("cat", 4 + g, 2 * i + 1)])
                deferred.append(pool_tail)
            while combines:
                combines.pop(0)()

        order = [("hist", 0)]
        for i in range(NOWN):
            order += [("ctx", i), ("own", i)]
        bufs = {}
        bufs[0] = a_begin(*order[0])
        a_tile(order[0][0], order[0][1], bufs[0], 0)
        bufs[1] = a_begin(*order[1])
        a_tile(order[1][0], order[1][1], bufs[1], 0)
        for gi, (name, c0, dst, d0) in enumerate(WGRP):
            P.op(("sp", "act")[gi % 2], lambda e, c0=c0: e.dma_start(out=WST[:, :, c0:c0 + 512], in_=w_in_v[:, :, c0:c0 + 512]),
                 writes=[("wst", name)], dma=True)
        ci = 0
        for name, c0, dst, d0 in WGRP:
            for kc in range(8):
                eng = "dve"
                ci += 1
                src = WST[:, kc, c0:c0 + 512]
                if eng == "act":
                    fn = lambda e, dst=dst, kc=kc, src=src, d0=d0: e.activation(
                        out=dst[:, kc, d0:d0 + 512], in_=src, func=AF.Copy, scale=gcol[:, kc:kc + 1])
                else:
                    fn = lambda e, dst=dst, kc=kc, src=src, d0=d0: e.tensor_scalar(
                        out=dst[:, kc, d0:d0 + 512], in0=src, scalar1=gcol[:, kc:kc + 1], scalar2=None, op0=ALU.mult)
                P.op(eng, fn, reads=[("wst", name), "gcol"], writes=[(name, kc)])
        fence([("wst", name) for name, _, _, _ in WGRP], kt_ids)
        for h in range(8):
            P.op("pool", lambda e, h=h: e.dma_start(out=KT[64:84, h, :], in_=kaug), writes=[("kaug", h)], dma=True)
        for n in range(len(order)):
            nxt = order[n + 1] if n + 1 < len(order) else None
            if nxt and n > 0:
                bufs[n + 1] = a_begin(*nxt)
                a_tile(nxt[0], nxt[1], bufs[n + 1], 0)
            late = a_tile(nxt[0], nxt[1], bufs[n + 1], 1, split=True) if nxt else None
            stage_b1(order[n][0], order[n][1], bufs[n])
            if late:
                late()
            stage_b1v(order[n][0], order[n][1], bufs[n])
            stage_b2(order[n][0], order[n][1], bufs[n])
        flush_deferred()

        p1_ids = (["hb", "fx", "cntfix", "wpool", "pscale", "pscw", "gcol"]
                  + [("sS", n_) for n_ in range(3)] + [("mix", g) for g in range(4)]
                  + [("xt", s) for s in range(2)] + [("hT", b, t) for b in range(2) for t in range(2)]
                  + [("ub", s) for s in range(2)] + [(("ub", s), "h") for s in range(2)]
                  + [("uH", g) for g in range(4)]
                  + [("ksum", h, ks) for h in range(8) for ks in range(16)]
                  + [(w_, kc) for w_ in ("wq", "wk", "wv", "wu") for kc in range(8)])
        cv.off = persist_end
        PT = [cv.bf(512) for _ in range(4)]
        atok = [cv.bf(1024).rearrange("p (t c) -> p t c", t=2) for _ in range(2)]
        rec = [cv.f32(2) for _ in range(2)]
        gm = cv.f32(128)
        m8 = cv.f32(64)
        stage = [cv.bf(128), cv.bf(128)]
        p3_ids = ([("PT", n) for n in range(4)] + [("atok", n) for n in range(2)] + [("rec", n) for n in range(2)]
                  + [("gmh", h) for h in range(8)] + [("m8", h) for h in range(8)] + [("stage", t_) for t_ in range(2)]
                  + [("cat", c, t) for c in range(4) for t in range(NT)] + [("wout", kc) for kc in range(8)])
        fence(p1_ids, p3_ids)
        for kc in range(8):
            P.op("pool", lambda e, kc=kc: e.dma_start(out=WOUT[:, kc, :], in_=w_out[kc * 128:(kc + 1) * 128, :]),
                 writes=[("wout", kc)], dma=True)

        units = []
        for i in range(NOWN):
            for h in range(H):
                wmax = int(np.floor((ALIBI_CUT / SLOPES[h] - 1.0) / 256.0 - 1e-9))
                for u in range(2 * i + 1):
                    gap = 2 * i - u
                    if gap <= wmax:
                        units.append((i, h, u))
                units.append((i, h, "diag"))
        st_ctr = [0]
        po_state = {}

        def emit_qk(unit):
            i, h, u = unit
            n = st_ctr[0] % 4
            nb_ = st_ctr[0] % 3
            st_ctr[0] += 1
            stv = psb[nb_]
            sid = fb_(nb_)
            qsl = QT[0:84, h, i * 256:(i + 1) * 256]
            qreads = [("qT", h, i), ("qM", 2 * i), ("qM", 2 * i + 1), "qaug"]
            if u != "diag":
                ks = u
                for c in range(2):
                    P.op("pe", lambda e, c=c: e.matmul(out=stv[:, c * 256:(c + 1) * 256],
                                                       lhsT=KT[0:84, h, ks * 256 + c * 128:ks * 256 + (c + 1) * 128],
                                                       rhs=qsl, start=True, stop=True),
                         reads=[("kT", h, ks), ("kaug", h)] + qreads, writes=sid)
                width = 512
            else:
                ks = 2 * i + 1
                k0 = KT[0:84, h, ks * 256:ks * 256 + 128]
                k1 = KT[0:84, h, ks * 256 + 128:ks * 256 + 256]
                rd = [("kT", h, ks), ("kaug", h)] + qreads
                P.op("pe", lambda e: e.matmul(out=stv[:, 0:128], lhsT=k0, rhs=qsl[:, 0:128], start=True, stop=False),
                     reads=rd, writes=sid)
                P.op("pe", lambda e: e.matmul(out=stv[:, 0:128], lhsT=ident, rhs=tri, start=False, stop=True),
                     reads=["ident", "tri"], writes=sid)
                P.op("pe", lambda e: e.matmul(out=stv[:, 128:256], lhsT=k0, rhs=qsl[:, 128:256], start=True, stop=True),
                     reads=rd, writes=sid)
                P.op("pe", lambda e: e.matmul(out=stv[:, 256:384], lhsT=k1, rhs=qsl[:, 128:256], start=True, stop=False),
                     reads=rd, writes=sid)
                P.op("pe", lambda e: e.matmul(out=stv[:, 256:384], lhsT=ident, rhs=tri, start=False, stop=True),
                     reads=["ident", "tri"], writes=sid)
                width = 384
            ptv = PT[n]
            P.op("act", lambda e: e.activation(out=ptv[:, 0:width], in_=stv[:, 0:width], func=AF.Exp),
                 reads=sid, writes=[("PT", n)])
            return n

        def emit_pv(unit, n):
            i, h, u = unit
            key = (i, h)
            if key not in po_state:
                b = len(po_state) % 2
                po_state[key] = dict(bank=b, started=False)
            stt = po_state[key]
            b = stt["bank"]
            pov = psb[4 + b]
            poid = fb_(4 + b)
            ptv = PT[n]
            if u != "diag":
                ks = u
                jobs = [(0, 0, ks * 2), (1, 128, ks * 2), (0, 256, ks * 2 + 1), (1, 384, ks * 2 + 1)]
                last = [False] * 4
            else:
                ks = 2 * i + 1
                jobs = [(0, 0, ks * 2), (1, 128, ks * 2), (1, 256, ks * 2 + 1)]
                last = [False, False, True]
            for (tt, c0, chunk), lst in zip(jobs, last):
                first = not stt["started"]
                stt["started"] = True
                P.op("pe", lambda e, tt=tt, c0=c0, chunk=chunk, first=first, lst=lst: e.matmul(
                    out=pov[:, tt * 65:(tt + 1) * 65], lhsT=ptv[:, c0:c0 + 128], rhs=VP[:, chunk, h, :],
                    start=first, stop=lst),
                    reads=[("PT", n), ("V", chunk), "vones"], writes=poid)
            if u == "diag":
                ab = i % 2
                rc = rec[b]
                P.op("dve", lambda e: e.reciprocal(out=rc, in_=pov[:, 0:130].rearrange("p (t c) -> p t c", t=2)[:, :, 64]),
                     reads=poid, writes=[("rec", b)])
                for tt in range(2):
                    P.op("dve", lambda e, tt=tt: e.tensor_scalar(
                        out=atok[ab][:, tt, h * 64:(h + 1) * 64], in0=pov[:, tt * 65:tt * 65 + 64],
                        scalar1=rc[:, tt:tt + 1], scalar2=None, op0=ALU.mult),
                        reads=poid + [("rec", b)], writes=[("atok", ab)])
                if h == H - 1:
                    def cat_tail(i=i, ab=ab):
                        trv, trid = tr3_bank()
                        for tt in range(2):
                            for cc in range(4):
                                P.op("pe", lambda e, tt=tt, cc=cc: e.transpose(
                                    out=trv[:, cc * 256 + tt * 128:cc * 256 + (tt + 1) * 128],
                                    in_=atok[ab][:, tt, cc * 128:(cc + 1) * 128], identity=ident),
                                    reads=[("atok", ab), "ident"], writes=trid)
                        P.op("dve", lambda e: e.tensor_copy(out=CAT[:, 0:4, i * 256:(i + 1) * 256],
                                                            in_=trv.rearrange("p (c s) -> p c s", c=4)),
                             reads=trid, writes=[("cat", c, 2 * i + t_) for c in range(4) for t_ in range(2)])
                    cat_pending.append([6, cat_tail])

        tr3_ctr = [0]
        cat_pending = []

        def cat_tick(force=False):
            for ent in list(cat_pending):
                ent[0] -= 1
                if ent[0] <= 0 or force:
                    cat_pending.remove(ent)
                    ent[1]()

        def tr3_bank():
            n = tr3_ctr[0] % 2
            tr3_ctr[0] += 1
            return psb[6 + n][:, :].bitcast(BF16), fb_(6 + n)

        gate_pending = {}
        gate_chains = {}

        def gate_chain(i, tt):
            gate_chains[i][tt]()

        def gate_front(i):
            tails = []
            gid = fb_(3)
            for tt in range(2):
                t = 2 * i + tt
                gvf = psb[3][:, tt * 128:(tt + 1) * 128]
                for h in range(8):
                    P.op("pe", lambda e, gvf=gvf, h=h, t=t: e.matmul(
                        out=gvf[:, h * 16:(h + 1) * 16], lhsT=QT[0:64, h, t * 128:(t + 1) * 128],
                        rhs=kmT[0:64, h * 16:(h + 1) * 16], start=True, stop=True),
                        reads=[("qT", h, i)] + [("kmT", s_) for s_ in range(16)], writes=gid)
            chains = []
            for tt in range(2):
              def chain(tt=tt):
                t = 2 * i + tt
                gvf = psb[3][:, tt * 128:(tt + 1) * 128]
                gmv = gm.rearrange("p (h s) -> p h s", h=8)
                elb = elig[:, i, :].unsqueeze(1).to_broadcast([128, 8, 16])
                owb = owntab[:, i, :].unsqueeze(1).to_broadcast([128, 8, 16])
                stg = stage[tt]
                P.op("dve", lambda e, gvf=gvf, gmv=gmv, elb=elb: e.tensor_tensor(
                    out=gmv, in0=gvf[:, 0:128].rearrange("p (h s) -> p h s", h=8), in1=elb, op=ALU.add),
                    reads=gid + ["elig"], writes=GMH)
                for h in range(8):
                    P.op("dve", lambda e, h=h: e.max(out=m8[:, h * 8:(h + 1) * 8], in_=gm[:, h * 16:(h + 1) * 16]),
                         reads=[("gmh", h)], writes=[("m8", h)])
                for h in range(8):
                    P.op("dve", lambda e, h=h: e.tensor_scalar(
                        out=gm[:, h * 16:(h + 1) * 16], in0=gm[:, h * 16:(h + 1) * 16],
                        scalar1=m8[:, h * 8 + 2:h * 8 + 3], scalar2=-BIG, op0=ALU.is_lt, op1=ALU.mult),
                        reads=[("gmh", h), ("m8", h)], writes=[("gmh", h)])
                P.op("dve", lambda e, gmv=gmv, elb=elb: e.tensor_tensor(out=gmv, in0=gmv, in1=elb, op=ALU.add),
                     reads=["elig"] + GMH, writes=GMH)
                P.op("dve", lambda e, gmv=gmv, owb=owb, stg=stg: e.tensor_tensor(
                    out=stg.rearrange("p (h s) -> p h s", h=8), in0=gmv, in1=owb, op=ALU.max),
                    reads=GMH + ["owntab"], writes=[("stage", tt)])

                def tail(t=t, tt=tt, stg=stg):
                    trv, trid = tr3_bank()
                    for h in range(8):
                        P.op("pe", lambda e, h=h: e.transpose(out=trv[0:16, h * 128:(h + 1) * 128],
                                                              in_=stg[:, h * 16:(h + 1) * 16], identity=ident),
                             reads=[("stage", tt), "ident"], writes=trid)
                    P.op("dve", lambda e: e.tensor_copy(out=QT[64:80, :, t * 128:(t + 1) * 128],
                                                        in_=trv[0:16, :].rearrange("p (h s) -> p h s", h=8)),
                         reads=trid, writes=[("qM", t)])
                tails.append(tail)
              chains.append(chain)
            gate_pending[i] = tails
            gate_chains[i] = chains

        def gate_tail(i):
            for f_ in gate_pending.pop(i):
                f_()

        gate_front(0)
        gate_chain(0, 0)
        gate_chain(0, 1)
        gate_tail(0)
        pend = []
        prev_ih = None
        for unit in units:
            ui, uh, uu = unit
            if (ui, uh) != prev_ih and ui + 1 < NOWN:
                if uh == 3:
                    gate_front(ui + 1)
                elif uh == 4:
                    gate_chain(ui + 1, 0)
                elif uh == 5:
                    gate_chain(ui + 1, 1)
                elif uh == 7:
                    gate_tail(ui + 1)
            prev_ih = (ui, uh)
            n = emit_qk(unit)
            pend.append((unit, n))
            if len(pend) > 2:
                emit_pv(*pend.pop(0))
            cat_tick()
        while pend:
            emit_pv(*pend.pop(0))
        cat_tick(force=True)

        if debug:
            P.op("sp", lambda e: e.dma_start(out=dbg["qt"][0:84, :], in_=RBC[0:84, 16640:33024]),
                 reads=[("qT", h, i) for h in range(8) for i in range(8)] + [("qM", t) for t in range(NT)] + ["qaug"],
                 dma=True)
            P.op("sp", lambda e: e.dma_start(out=dbg["kt"][0:84, :], in_=RA[0:84, :]), reads=kt_ids, dma=True)
            P.op("sp", lambda e: e.dma_start(out=dbg["vp"], in_=RBC[:, 0:16640]),
                 reads=[("V", c) for c in range(32)] + ["vones"], dma=True)

        if debug:
            P.op("sp", lambda e: e.dma_start(out=dbg["cat"], in_=RD[:, :]),
                 reads=[("cat", c, t) for c in range(8) for t in range(NT)], dma=True)

        p3_old = ([("PT", n) for n in range(4)] + [("atok", n) for n in range(2)] + [("rec", n) for n in range(2)]
                  + [("gmh", h) for h in range(8)] + [("m8", h) for h in range(8)] + [("stage", t_) for t_ in range(2)]
                  + [("kmT", ks) for ks in range(16)] + ["elig", "owntab"]
                  + kt_ids + [("V", c) for c in range(32)] + ["vones", "qaug"]
                  + [("qT", h, i) for h in range(8) for i in range(8)] + [("qM", t) for t in range(NT)])
        cv.off = persist_end
        gb = cv.f32(1024)
        hb4 = [cv.bf(1024), cv.bf(1024), cv.bf(1024)]
        actT = [cv.bf(2048).rearrange("p (f s) -> p f s", f=8) for _ in range(2)]
        rtmp = [cv.f32(256) for _ in range(2)]
        xr = [cv.f32(1024), cv.f32(1024)]
        p4_ids = (["gb", ("hb4", 0), ("hb4", 1), ("hb4", 2)] + [("actT", n, fb) for n in range(2) for fb in range(8)] + [("rtmp", n) for n in range(2)]
                  + [("xr", n) for n in range(2)] + [("xm", t, c) for t in range(NT) for c in range(2)]
                  + [("wup", s, kc) for s in range(2) for kc in range(8)]
                  + [("wdn", s, kc) for s in range(2) for kc in range(8)])
        fence(p3_old, p4_ids)

        def load_ffn(fg):
            s = fg % 2
            wu, wd = ring(s)
            for kc in range(8):
                P.op("pool", lambda e, kc=kc: e.dma_start(
                    out=wu[:, kc, :], in_=w_up[kc * 128:(kc + 1) * 128, fg * 1024:(fg + 1) * 1024]),
                    writes=[("wup", s, kc)], dma=True)
            for fb in range(8):
                P.op("pool", lambda e, fb=fb: e.dma_start(
                    out=wd[:, fb, :], in_=w_down[fg * 1024 + fb * 128:fg * 1024 + (fb + 1) * 128, :]),
                    writes=[("wdn", s, fb)], dma=True)

        load_ffn(0)
        load_ffn(1)
        P.op("sp", lambda e: e.dma_start(out=gb, in_=g_mlp.partition_broadcast(128)), writes=["gb"], dma=True)

        tr4_ctr = [0]

        def wout_mm(t):
            slot = t % 2
            P.op("sp", lambda e: e.dma_start(out=xr[slot], in_=x_own[t * 128:(t + 1) * 128, :]),
                 writes=[("xr", slot)], dma=True)
            n = t % 3
            for ch in range(2):
                bank = psb[2 * n + ch]
                bid = fb_(2 * n + ch)
                for kc in range(8):
                    P.op("pe", lambda e, bank=bank, kc=kc, ch=ch: e.matmul(
                        out=bank[:, :], lhsT=CAT[:, kc, t * 128:(t + 1) * 128], rhs=WOUT[:, kc, ch * 512:(ch + 1) * 512],
                        start=(kc == 0), stop=(kc == 7)),
                        reads=[("cat", kc, t), ("wout", kc)], writes=bid)
                P.op("dve", lambda e, bank=bank, ch=ch: e.tensor_tensor(
                    out=XM[:, t, ch * 512:(ch + 1) * 512], in0=bank[:, :], in1=xr[slot][:, ch * 512:(ch + 1) * 512], op=ALU.add),
                    reads=bid + [("xr", slot)], writes=[("xm", t, ch)])
            col = 34 + t
            norm_rs(col, XM[:, t, :], [("xm", t, 0), ("xm", t, 1)], hb4[t % 3], ("hb4", t % 3))
            P.op("dve", lambda e: e.scalar_tensor_tensor(out=hb4[t % 3], in0=XM[:, t, :], scalar=rs[:, col:col + 1], in1=gb,
                                                         op0=ALU.mult, op1=ALU.mult),
                 reads=[("xm", t, 0), ("xm", t, 1), ("rs", col), "gb"], writes=[("hb4", t % 3)])

        def wout_tail(t):
            k = tr4_ctr[0] % 2
            tr4_ctr[0] += 1
            trv = psb[6 + k][:, :].bitcast(BF16)
            trid = fb_(6 + k)
            for kc in range(8):
                P.op("pe", lambda e, kc=kc: e.transpose(out=trv[:, kc * 128:(kc + 1) * 128],
                                                        in_=hb4[t % 3][:, kc * 128:(kc + 1) * 128], identity=ident),
                     reads=[("hb4", t % 3), "ident"], writes=trid)
            P.op("act", lambda e: e.copy(out=CAT[:, :, t * 128:(t + 1) * 128], in_=trv.rearrange("p (k s) -> p k s", k=8)),
                 reads=trid, writes=[("cat", c, t) for c in range(8)])

        for t in range(NT):
            wout_mm(t)
            if t >= 2:
                wout_tail(t - 2)
        ffn_inject = {0: [lambda: wout_tail(NT - 2)], 1: [lambda: wout_tail(NT - 1)]}

        pu_ctr = [0]
        pd_ctr = [0]
        act_ctr = [0]
        rt_ctr = [0]

        def ffn_up(fg, tp):
            s = fg % 2
            wu, _ = ring(s)
            ab = act_ctr[0] % 2
            act_ctr[0] += 1
            for fb in range(8):
                n = pu_ctr[0] % 4
                pu_ctr[0] += 1
                pv = psb[n][:, 0:256]
                pid = ("ps", n)
                for kc in range(8):
                    P.op("pe", lambda e, pv=pv, kc=kc, fb=fb: e.matmul(
                        out=pv, lhsT=wu[:, kc, fb * 128:(fb + 1) * 128], rhs=CAT[:, kc, tp * 256:(tp + 1) * 256],
                        start=(kc == 0), stop=(kc == 7)),
                        reads=[("wup", s, kc), ("cat", kc, 2 * tp), ("cat", kc, 2 * tp + 1)], writes=[pid])
                r = rt_ctr[0] % 2
                rt_ctr[0] += 1
                P.op("act", lambda e, pv=pv, r=r: e.activation(out=rtmp[r], in_=pv, func=AF.Relu),
                     reads=[pid], writes=[("rtmp", r)])
                P.op("dve", lambda e, r=r, fb=fb, ab=ab: e.tensor_tensor(out=actT[ab][:, fb, :], in0=rtmp[r], in1=rtmp[r],
                                                                       op=ALU.mult),
                     reads=[("rtmp", r)], writes=[("actT", ab, fb)])
            return ab

        def ffn_down(fg, tp, ab):
            s = fg % 2
            _, wd = ring(s)
            for tt in range(2):
                t = 2 * tp + tt
                for ch in range(2):
                    n = pd_ctr[0] % 4
                    pd_ctr[0] += 1
                    bank = psb[4 + n]
                    bid = fb_(4 + n)
                    for fb in range(8):
                        P.op("pe", lambda e, bank=bank, fb=fb, tt=tt, ch=ch: e.matmul(
                            out=bank[:, :], lhsT=actT[ab][:, fb, tt * 128:(tt + 1) * 128],
                            rhs=wd[:, fb, ch * 512:(ch + 1) * 512], start=(fb == 0), stop=(fb == 7)),
                            reads=[("actT", ab, fb), ("wdn", s, fb)], writes=bid)
                    P.op("dve", lambda e, bank=bank, t=t, ch=ch: e.tensor_tensor(
                        out=XM[:, t, ch * 512:(ch + 1) * 512], in0=bank[:, :], in1=XM[:, t, ch * 512:(ch + 1) * 512],
                        op=ALU.add),
                        reads=bid + [("xm", t, ch)], writes=[("xm", t, ch)])

        out_ops = []

        def final_tile(t):
            col = 50 + t
            norm_rs(col, XM[:, t, :], [("xm", t, 0), ("xm", t, 1)], hb4[t % 3], ("hb4", t % 3))
            slot = t % 2
            P.op("dve", lambda e: e.scalar_tensor_tensor(out=xr[slot], in0=XM[:, t, :], scalar=rs[:, col:col + 1], in1=gb,
                                                         op0=ALU.mult, op1=ALU.mult),
                 reads=[("xm", t, 0), ("xm", t, 1), ("rs", col), "gb"], writes=[("xr", slot)])
            out_ops.append(P.op("sp", lambda e: e.dma_start(out=y[t * 128:(t + 1) * 128, :], in_=xr[slot]),
                                reads=[("xr", slot)], dma=True))

        seq = [(fg, tp) for fg in range(4) for tp in range(8)]
        prev = None
        loaded = 2
        gfin_loaded = False
        for idx, (fg, tp) in enumerate(seq):
            ab = ffn_up(fg, tp)
            for f_ in ffn_inject.pop(idx, []):
                f_()
            if prev is not None:
                pfg, ptp, pab = prev
                ffn_down(pfg, ptp, pab)
                if ptp == 7 and pfg + 2 < 4:
                    load_ffn(pfg + 2)
                if pfg == 3:
                    if not gfin_loaded:
                        P.op("sp", lambda e: e.dma_start(out=gb, in_=g_fin.partition_broadcast(128)), writes=["gb"], dma=True)
                        gfin_loaded = True
                    final_tile(2 * ptp)
                    final_tile(2 * ptp + 1)
            prev = (fg, tp, ab)
        pfg, ptp, pab = prev
        ffn_down(pfg, ptp, pab)
        final_tile(2 * ptp)
        final_tile(2 * ptp + 1)

        dbg_ops = [o for o in P.ops if o.dma and o.eng == "sp" and not o.writes and o not in out_ops]
        P.op("sp", lambda e: None, after=out_ops + dbg_ops)
        stats = P.emit(st)
    return nc, stats


def _core_tables(r):
    bf = ml_dtypes.bfloat16
    own_g = [2 * i + r for i in range(NOWN)]
    ctx_g = [(2 * i - 1 + r) % NBLK for i in range(NOWN)]
    slot_g = []
    for i in range(NOWN):
        slot_g += [ctx_g[i], own_g[i]]
    kaug = np.zeros((20, 4096), np.float32)
    for ks in range(16):
        sl = slice(ks * 256, (ks + 1) * 256)
        kaug[ks, sl] = 1.0
        kaug[16, sl] = 1.0
        kaug[17, sl] = 1.0
        kaug[18, sl] = np.arange(256)
        kaug[19, sl] = slot_g[ks]
    qaug = np.zeros((4, H, NTOK), np.float32)
    for h in range(H):
        s = SLOPES[h]
        for i in range(NOWN):
            sl = slice(i * 256, (i + 1) * 256)
            qaug[0, h, sl] = -s * np.arange(256)
            qaug[1, h, sl] = -s * 256.0 * own_g[i]
            qaug[2, h, sl] = s
            qaug[3, h, sl] = s * 256.0
    elig = np.zeros((128, NOWN, 16), np.float32)
    owntab = np.full((128, NOWN, 16), -3.0 * BIG, np.float32)
    for i in range(NOWN):
        for ks in range(16):
            elig[:, i, ks] = 0.0 if slot_g[ks] < own_g[i] else -BIG
        owntab[:, i, 2 * i + 1] = 0.0
    tri = np.where(np.arange(128)[:, None] <= np.arange(128)[None, :], 0.0, -BIG).astype(np.float32)
    ident = np.eye(128, dtype=np.float32)
    cntfix = np.zeros((128, 4, 16), np.float32)
    for g, w in enumerate(WINS):
        pos = own_g[0] * 256 + np.arange(16)
        cntfix[:, g, :] = float(w) / np.minimum(pos + 1, w)
    return dict(kaug=kaug.astype(bf), qaug=qaug.reshape(4, H * NTOK).astype(bf),
                elig=elig.reshape(128, -1).astype(bf), owntab=owntab.reshape(128, -1).astype(bf),
                tri=tri.astype(bf), ident=ident.astype(bf), cntfix=cntfix.reshape(128, 64)), own_g, ctx_g


_CACHE = {}


def kernel(x, norm_mix, w_in, w_pool, pool_scale, w_out, norm_mlp, w_up, w_down, norm_final, _debug=False):
    x = np.ascontiguousarray(np.asarray(x, dtype=np.float32))
    f = lambda a: np.ascontiguousarray(np.asarray(a, dtype=np.float32))
    key = ("nc", bool(_debug))
    if key not in _CACHE:
        _CACHE[key] = build_program(debug=_debug)
    nc, stats = _CACHE[key]
    shared = dict(
        w_in=f(w_in)[0], w_out=f(w_out)[0], w_up=f(w_up)[0], w_down=f(w_down)[0],
        w_pool=f(w_pool)[0].reshape(512, 128),
        pscale_t=np.ascontiguousarray(f(pool_scale)[0].reshape(4, 128).T),
        gmix_t=np.ascontiguousarray(f(norm_mix)[0].reshape(8, 128).T),
        g_mlp=f(norm_mlp)[0], g_fin=f(norm_final),
    )
    in_maps = []
    meta = []
    for c in range(NCORES):
        b, r = divmod(c, 2)
        tabs, own_g, ctx_g = _core_tables(r)
        xb = x[b].reshape(NBLK, BLK, D)
        x_own = np.ascontiguousarray(xb[own_g].reshape(NTOK, D))
        x_ctx = np.ascontiguousarray(xb[ctx_g].reshape(NTOK, D))
        x_hist = np.zeros((NOWN, 16, D), np.float32)
        for i, g in enumerate(own_g):
            if g > 0:
                x_hist[i] = x[b, g * BLK - 16:g * BLK]
        m = dict(shared)
        m.update(tabs)
        m.update(x_own=x_own, x_ctx=x_ctx, x_hist=x_hist.reshape(128, D))
        in_maps.append(m)
        meta.append((b, own_g))
    res = run_bass_kernel_spmd(nc, in_maps, core_ids=list(range(NCORES)))
    out = np.empty((NBATCH, SEQ, D), np.float32)
    for c in range(NCORES):
        b, own_g = meta[c]
        yb = np.asarray(res.results[c]["y"]).reshape(NOWN, BLK, D)
        ob = out[b].reshape(NBLK, BLK, D)
        for i, g in enumerate(own_g):
            ob[g] = yb[i]
    if _debug:
        return out, res
    return out
```

```python
import numpy as np
import ml_dtypes
from contextlib import ExitStack
import concourse.bass as bass
import concourse.mybir as mybir
from concourse.bass_utils import run_bass_kernel_spmd

F32 = mybir.dt.float32
BF16 = mybir.dt.bfloat16
ALU = mybir.AluOpType
AF = mybir.ActivationFunctionType

ENGS = ("pe", "act", "dve", "pool", "sp")


class Op:
    __slots__ = ("eng", "fn", "reads", "writes", "dma", "idx", "deps", "waits",
                 "signal", "dom", "val", "clock", "extra")

    def __init__(self, eng, fn, reads, writes, dma):
        self.eng = eng
        self.fn = fn
        self.reads = tuple(reads)
        self.writes = tuple(writes)
        self.dma = dma
        self.signal = False
        self.waits = []
        self.deps = []
        self.extra = []
        self.idx = 0
        self.val = 0


class Prog:
    def __init__(self, nc, n_dma_sems=20):
        self.nc = nc
        self.ops = []
        self.n_dma_sems = n_dma_sems

    def op(self, eng, fn, reads=(), writes=(), dma=False, after=()):
        o = Op(eng, fn, reads, writes, dma)
        o.extra = list(after)
        self.ops.append(o)
        return o

    def _analyze(self):
        last_write = {}
        reads_since = {}
        eng_count = {e: 0 for e in ENGS}
        dma_rr = {e: 0 for e in ENGS}
        dma_last = {}
        dma_val = {}
        for o in self.ops:
            if o.dma:
                k = dma_rr[o.eng] % self.n_dma_sems
                dma_rr[o.eng] += 1
                o.dom = ("dma", o.eng, k)
                o.val = dma_val.get(o.dom, 0) + 16
                dma_val[o.dom] = o.val
                prev = dma_last.get(o.dom)
                if prev is not None:
                    o.deps.append(prev)
                dma_last[o.dom] = o
            else:
                o.dom = o.eng
                eng_count[o.eng] += 1
                o.idx = eng_count[o.eng]
            deps = list(o.extra)
            for b in o.reads:
                lw = last_write.get(b)
                if lw:
                    deps.extend(lw.values())
            for b in o.writes:
                lw = last_write.get(b)
                if lw:
                    deps.extend(lw.values())
                rs = reads_since.get(b)
                if rs:
                    deps.extend(rs.values())
            seen = set()
            for d in deps:
                if d is o or id(d) in seen:
                    continue
                seen.add(id(d))
                if (not d.dma) and (not o.dma) and d.eng == o.eng:
                    if o.eng == "pe":
                        continue
                o.deps.append(d)
            for b in o.reads:
                reads_since.setdefault(b, {})[o.dom] = o
            for b in o.writes:
                last_write[b] = {o.dom: o}
                reads_since[b] = {}
        know = {e: {} for e in ENGS}
        for o in self.ops:
            kn = know[o.eng]
            for d in o.deps:
                dv = d.val if d.dma else d.idx
                if (not d.dma) and d.eng == o.eng:
                    key = ("self", d.dom)
                    if kn.get(key, 0) >= dv:
                        continue
                    kn[key] = dv
                    o.waits.append(d)
                    d.signal = True
                    continue
                if kn.get(d.dom, 0) >= dv:
                    continue
                o.waits.append(d)
                d.signal = True
                kn[d.dom] = dv
                for k2, v2 in d.clock.items():
                    if isinstance(k2, tuple) and k2[0] == "self":
                        continue
                    if kn.get(k2, 0) < v2:
                        kn[k2] = v2
            o.clock = dict(kn)
            if not o.dma:
                o.clock[o.dom] = o.idx
        cnt = {e: 0 for e in ENGS}
        for o in self.ops:
            if o.dma:
                o.signal = True
                continue
            if o.signal:
                cnt[o.eng] += 1
                o.val = cnt[o.eng]

    def emit(self, stack):
        nc = self.nc
        self._analyze()
        sems = {}
        for e in ENGS:
            sems[e] = stack.enter_context(nc.semaphore("s_" + e))
        for o in self.ops:
            if o.dma and o.dom not in sems:
                sems[o.dom] = stack.enter_context(
                    nc.semaphore("d_%s_%d" % (o.dom[1], o.dom[2])))
        block = stack.enter_context(nc.Block())
        by_eng = {e: [o for o in self.ops if o.eng == e] for e in ENGS}

        def run(eng_name):
            def body(eng):
                for o in by_eng[eng_name]:
                    for d in o.waits:
                        eng.wait_ge(sems[d.dom], d.val)
                    ins = o.fn(eng)
                    if o.signal and ins is not None:
                        ins.then_inc(sems[o.dom], 16 if o.dma else 1)
            return body

        block.tensor(run("pe"))
        block.scalar(run("act"))
        block.vector(run("dve"))
        block.gpsimd(run("pool"))
        block.sync(run("sp"))
        return dict(n_ops=len(self.ops), n_waits=sum(len(o.waits) for o in self.ops),
                    per_eng={e: len(by_eng[e]) for e in ENGS})


D = 1024
SEQ = 4096
NBATCH = 4
H = 8
DH = 64
BLK = 256
NBLK = 16
NOWN = 8
NTOK = NOWN * BLK
NT = NTOK // 128
DFF = 4096
EPS = 1e-6
BIG = 30000.0
ALIBI_CUT = 45.0
WINS = (2, 4, 8, 16)
SLOPES = [2.0 ** (-(h + 1)) for h in range(H)]
NCORES = 8


def build_program(debug=False):
    nc = bass.Bass("TRN2", target_bir_lowering=False)

    def din(name, shape, dt=F32):
        return nc.dram_tensor(name, shape, dt, kind="ExternalInput").ap()

    x_own = din("x_own", [NTOK, D])
    x_ctx = din("x_ctx", [NTOK, D])
    x_hist = din("x_hist", [128, D])
    w_in = din("w_in", [D, 2048])
    w_out = din("w_out", [D, D])
    w_up = din("w_up", [D, DFF])
    w_down = din("w_down", [DFF, D])
    w_pool = din("w_pool", [512, 128])
    pscale_t = din("pscale_t", [128, 4])
    gmix_t = din("gmix_t", [128, 8])
    g_mlp = din("g_mlp", [D])
    g_fin = din("g_fin", [D])
    kaug = din("kaug", [20, 4096], BF16)
    qaug = din("qaug", [4, H * NTOK], BF16)
    elig_d = din("elig", [128, NOWN * 16], BF16)
    owntab_d = din("owntab", [128, NOWN * 16], BF16)
    tri_d = din("tri", [128, 128], BF16)
    ident_d = din("ident", [128, 128], BF16)
    cntfix_d = din("cntfix", [128, 64])
    y = nc.dram_tensor("y", [NTOK, D], F32, kind="ExternalOutput").ap()
    dbg = {}
    if debug:
        dbg["cat"] = nc.dram_tensor("dbg_cat", [128, 8 * NTOK], BF16, kind="ExternalOutput").ap()
        dbg["qt"] = nc.dram_tensor("dbg_qt", [128, 8 * NTOK], BF16, kind="ExternalOutput").ap()
        dbg["kt"] = nc.dram_tensor("dbg_kt", [128, 8 * 4096], BF16, kind="ExternalOutput").ap()
        dbg["vp"] = nc.dram_tensor("dbg_vp", [128, 32 * 8 * 65], BF16, kind="ExternalOutput").ap()

    with ExitStack() as st:
        RA = st.enter_context(nc.sbuf_tensor("RA", [128, 32768], BF16))
        RBC = st.enter_context(nc.sbuf_tensor("RBC", [128, 33024], BF16))
        RD = st.enter_context(nc.sbuf_tensor("RD", [128, 16384], BF16))
        RE = st.enter_context(nc.sbuf_tensor("RE", [128, 8192], BF16))
        NW = 16032
        RW = st.enter_context(nc.sbuf_tensor("RW", [128, NW], BF16))
        psb = [st.enter_context(nc.psum_tensor("ps%d" % b, [128, 512], F32)) for b in range(8)]

        KT = RA[:, :].rearrange("p (h s) -> p h s", h=8)
        WST = RA[:, :].bitcast(F32).rearrange("p (k n) -> p k n", k=8)
        XM = RA[:, :].bitcast(F32).rearrange("p (t d) -> p t d", t=16)
        VP = RBC[:, 0:16640].rearrange("p (c h e) -> p c h e", c=32, h=8)
        VP1 = RBC[:, 0:16640].rearrange("p (m e) -> p m e", e=65)
        QT = RBC[:, 16640:33024].rearrange("p (h s) -> p h s", h=8)
        CAT = RD[:, :].rearrange("p (c s) -> p c s", c=8)
        WQK = RD[:, 0:8192].rearrange("p (k n) -> p k n", k=8)
        WVU = RE[:, :].rearrange("p (k n) -> p k n", k=8)
        WOUT = WVU

        def ring(slot):
            base = slot * 16384
            wu = RBC[:, base:base + 8192].rearrange("p (k n) -> p k n", k=8)
            wd = RBC[:, base + 8192:base + 16384].rearrange("p (k n) -> p k n", k=8)
            return wu, wd

        class Carver:
            def __init__(self):
                self.off = 0

            def bf(self, n):
                a = RW[:, self.off:self.off + n]
                self.off += n
                assert self.off <= NW, self.off
                return a

            def f32(self, n):
                if self.off % 2:
                    self.off += 1
                a = RW[:, self.off:self.off + 2 * n].bitcast(F32)
                self.off += 2 * n
                assert self.off <= NW, self.off
                return a

        cv = Carver()
        tri = cv.bf(128)
        ident = cv.bf(128)
        ss = cv.f32(68)
        rs = cv.f32(68)
        dummy = cv.f32(16)
        epsb = cv.f32(2)
        elig = cv.bf(128).rearrange("p (i s) -> p i s", i=8)
        owntab = cv.bf(128).rearrange("p (i s) -> p i s", i=8)
        kmT = cv.bf(128)
        persist_end = cv.off

        cntfix = cv.f32(64)
        wpool = cv.bf(512).rearrange("p (g d) -> p g d", g=4)
        pscale = cv.f32(4)
        cv_pscw = cv.f32(4)
        gcol = cv.f32(8)
        ksum = cv.f32(128)
        fx = cv.f32(16)
        uH = cv.f32(512).rearrange("p (g s) -> p g s", g=4)
        ub = [cv.f32(272), cv.f32(272)]
        sS = [cv.f32(272) for _ in range(3)]
        mixT = [cv.bf(256) for _ in range(4)]
        hb = cv.bf(1024)
        hTb = [cv.bf(2048).rearrange("p (k s) -> p k s", k=8) for _ in range(2)]
        xt = [cv.f32(1024), cv.f32(1024)]
        p1_end = cv.off

        P = Prog(nc)

        def fb_(bank):
            return [("ps", bank)]
        dummy_i = [0]

        def fence(old_ids, new_ids):
            j = dummy_i[0] % 16
            dummy_i[0] += 1
            return P.op("pool", lambda e: e.memset(dummy[0:1, j:j + 1], 0.0),
                        writes=list(old_ids) + list(new_ids) + [("fx_dummy", j)])

        P.op("sp", lambda e: e.dma_start(out=tri, in_=tri_d), writes=["tri"], dma=True)
        P.op("sp", lambda e: e.dma_start(out=ident, in_=ident_d), writes=["ident"], dma=True)
        P.op("sp", lambda e: e.dma_start(out=gcol, in_=gmix_t), writes=["gcol"], dma=True)
        w_in_v = w_in.rearrange("(k p) n -> p k n", p=128)
        WGRP = [("wu", 1536, WVU, 512), ("wk", 512, WQK, 512), ("wv", 1024, WVU, 0), ("wq", 0, WQK, 0)]
        P.op("sp", lambda e: e.dma_start(out=cntfix, in_=cntfix_d), writes=["cntfix"], dma=True)
        P.op("sp", lambda e: e.dma_start(out=elig.rearrange("p i s -> p (i s)"), in_=elig_d), writes=["elig"], dma=True)
        P.op("sp", lambda e: e.dma_start(out=owntab.rearrange("p i s -> p (i s)"), in_=owntab_d), writes=["owntab"], dma=True)
        P.op("sp", lambda e: e.dma_start(out=pscale, in_=pscale_t), writes=["pscale"], dma=True)
        P.op("pool", lambda e: e.dma_start(out=wpool, in_=w_pool.rearrange("(g c) d -> c g d", g=4)),
             writes=["wpool"], dma=True)
        P.op("sp", lambda e: e.dma_start(out=QT[80:84, :, :], in_=qaug.rearrange("r (h s) -> r h s", h=8)),
             writes=["qaug"], dma=True)
        P.op("pool", lambda e: e.memset(VP1[:, :, 64:65], 1.0), writes=["vones"])
        P.op("pool", lambda e: e.memset(ksum, 0.0), writes=[("ksum", h, ks) for h in range(8) for ks in range(16)])
        P.op("pool", lambda e: e.memset(kmT, 0.0), writes=[("kmT", ks) for ks in range(16)])
        P.op("pool", lambda e: e.memset(ss, 0.0), writes=[("ss", c) for c in range(68)])
        P.op("pool", lambda e: e.memset(epsb, EPS), writes=["epsb"])

        pscw = cv_pscw
        for g in range(4):
            P.op("dve", lambda e, g=g: e.tensor_scalar(out=pscw[:, g:g + 1], in0=pscale[:, g:g + 1],
                                                      scalar1=1.0 / WINS[g], scalar2=None, op0=ALU.mult),
                 reads=["pscale"], writes=["pscw"])
        kt_ids = [("kT", h, ks) for h in range(8) for ks in range(16)] + [("kaug", h) for h in range(8)]
        tile_ctr = [0]
        blk_ctr = [0]
        pp_ctr = [0]
        pv_ctr = [0]
        tr_ctr = [0]
        ub_ctr = [0]
        mix_ctr = [0]

        def pp_slot():
            n = pp_ctr[0] % 4
            pp_ctr[0] += 1
            bank = 2 + n
            return psb[bank][:, 0:256], ("ps", bank)

        def tr_bank():
            n = tr_ctr[0] % 2
            tr_ctr[0] += 1
            return psb[n][:, :].bitcast(BF16), fb_(n)

        def norm_rs(col, src_ap, src_ids, junk, junk_id):
            P.op("act", lambda e: e.activation(out=junk, in_=src_ap, func=AF.Square, accum_out=ss[:, col:col + 1]),
                 reads=src_ids, writes=[junk_id, ("ss", col)])
            P.op("act", lambda e: e.activation(out=rs[:, col:col + 1], in_=ss[:, col:col + 1], func=AF.Ln,
                                               scale=1.0 / D, bias=epsb[:, 0:1]),
                 reads=[("ss", col), "epsb"], writes=[("rs", col)])
            P.op("act", lambda e: e.activation(out=rs[:, col:col + 1], in_=rs[:, col:col + 1], func=AF.Exp, scale=-0.5),
                 reads=[("rs", col)], writes=[("rs", col)])

        def a_begin(kind, i):
            bidx = blk_ctr[0]
            blk_ctr[0] += 1
            return bidx % 2

        def a_tile(kind, i, buf, tt, split=False):
            if kind == "hist" and tt == 1:
                return
            if True:
                tc_ = tile_ctr[0]
                tile_ctr[0] += 1
                slot = tc_ % 2
                col = tc_
                if kind == "hist":
                    src = x_hist
                elif kind == "ctx":
                    src = x_ctx[(i * 2 + tt) * 128:(i * 2 + tt + 1) * 128, :]
                else:
                    src = x_own[(i * 2 + tt) * 128:(i * 2 + tt + 1) * 128, :]
                xts = xt[slot]
                P.op("sp", lambda e, xts=xts, src=src: e.dma_start(out=xts, in_=src), writes=[("xt", slot)], dma=True)
                norm_rs(col, xts, [("xt", slot)], hb, "hb")
                P.op("act", lambda e, xts=xts, col=col: e.activation(out=hb, in_=xts, func=AF.Copy,
                                                                    scale=rs[:, col:col + 1]),
                     reads=[("xt", slot), ("rs", col)], writes=["hb"])

                def a_trans(buf=buf, tt=tt):
                    trv, trid = tr_bank()
                    for kc in range(8):
                        P.op("pe", lambda e, trv=trv, kc=kc: e.transpose(out=trv[:, kc * 128:(kc + 1) * 128],
                                                                         in_=hb[:, kc * 128:(kc + 1) * 128], identity=ident),
                             reads=["hb", "ident"], writes=trid)
                    P.op("dve", lambda e, trv=trv: e.tensor_copy(
                        out=hTb[buf][:, :, tt * 128:(tt + 1) * 128], in_=trv.rearrange("p (k s) -> p k s", k=8)),
                        reads=trid, writes=[("hT", buf, tt)])
                if split:
                    return a_trans
                a_trans()

        def proj(buf, wview, wname, c0, ntok):
            pv, pid = pp_slot()
            for kc in range(8):
                P.op("pe", lambda e, pv=pv, kc=kc: e.matmul(out=pv[:, 0:ntok], lhsT=wview[:, kc, c0:c0 + 128],
                                                            rhs=hTb[buf][:, kc, 0:ntok], start=(kc == 0), stop=(kc == 7)),
                     reads=[(wname, kc), ("hT", buf, 0), ("hT", buf, 1)], writes=[pid])
            return pv, pid

        deferred = []
        GMH = [("gmh", h) for h in range(8)]

        def flush_deferred():
            while deferred:
                deferred.pop(0)()

        gps_state = {}

        def stage_b1(kind, i, buf):
            if kind == "hist":
                for g in range(4):
                    pv, pid = proj(buf, WVU, "wu", 512 + g * 128, 128)
                    P.op("dve", lambda e, pv=pv, g=g: e.tensor_copy(out=uH[:, g, :], in_=pv[:, 0:128]),
                         reads=[pid], writes=[("uH", g)])
                return
            ks = 2 * i if kind == "ctx" else 2 * i + 1
            for cg in range(4):
                pv, pid = proj(buf, WQK, "wk", 512 + cg * 128, 256)
                for half in range(2):
                    h = 2 * cg + half
                    if half == 0:
                        P.op("act", lambda e, pv=pv, h=h, half=half: e.activation(
                            out=KT[0:64, h, ks * 256:(ks + 1) * 256], in_=pv[half * 64:(half + 1) * 64, 0:256],
                            func=AF.Copy, accum_out=ksum[0:64, h * 16 + ks:h * 16 + ks + 1]),
                            reads=[pid], writes=[("kT", h, ks), ("ksum", h, ks)])
                    else:
                        P.op("dve", lambda e, pv=pv, h=h, half=half: e.tensor_scalar(
                            out=KT[0:64, h, ks * 256:(ks + 1) * 256], in0=pv[half * 64:(half + 1) * 64, 0:256],
                            scalar1=1.0, scalar2=0.0, op0=ALU.mult, op1=ALU.add,
                            accum_out=ksum[0:64, h * 16 + ks:h * 16 + ks + 1]),
                            reads=[pid], writes=[("kT", h, ks), ("ksum", h, ks)])
            ksv = ksum.rearrange("p (h s) -> p h s", h=8)
            kmv = kmT.rearrange("p (h s) -> p h s", h=8)
            P.op("dve", lambda e: e.tensor_scalar(out=kmv[0:64, :, ks:ks + 1], in0=ksv[0:64, :, ks:ks + 1],
                                                  scalar1=1.0 / BLK, scalar2=None, op0=ALU.mult),
                 reads=[("ksum", h, ks) for h in range(8)], writes=[("kmT", ks)])

        def stage_b1v(kind, i, buf):
            if kind == "hist":
                return
            ks = 2 * i if kind == "ctx" else 2 * i + 1
            for tt in range(2):
                n = pv_ctr[0] % 2
                pv_ctr[0] += 1
                pvv = psb[6 + n]
                pvid = fb_(6 + n)
                for kc in range(8):
                    P.op("pe", lambda e, pvv=pvv, kc=kc, tt=tt: e.matmul(
                        out=pvv[:, :], lhsT=hTb[buf][:, kc, tt * 128:(tt + 1) * 128], rhs=WVU[:, kc, 0:512],
                        start=(kc == 0), stop=(kc == 7)),
                        reads=[("wv", kc), ("hT", buf, tt)], writes=pvid)
                chunk = ks * 2 + tt
                P.op("dve", lambda e, pvv=pvv, chunk=chunk: e.tensor_copy(
                    out=VP[:, chunk, :, 0:64], in_=pvv[:, :].rearrange("p (h d) -> p h d", h=8)),
                    reads=pvid, writes=[("V", chunk)])
            flush_deferred()

        def stage_b2(kind, i, buf):
            if kind != "own":
                return
            for cg in range(4):
                pv, pid = proj(buf, WQK, "wq", cg * 128, 256)
                for half in range(2):
                    h = 2 * cg + half
                    P.op("dve", lambda e, pv=pv, h=h, half=half: e.tensor_scalar(
                        out=QT[0:64, h, i * 256:(i + 1) * 256], in0=pv[half * 64:(half + 1) * 64, 0:256],
                        scalar1=DH ** -0.5, scalar2=None, op0=ALU.mult),
                        reads=[pid], writes=[("qT", h, i)])
            combines = []
            for g in range(4):
                pv, pid = proj(buf, WVU, "wu", 512 + g * 128, 256)
                us = ub_ctr[0] % 2
                ub_ctr[0] += 1
                u_ = ub[us]
                uid = ("ub", us)
                P.op("dve", lambda e, pv=pv, u_=u_: e.tensor_copy(out=u_[:, 16:272], in_=pv[:, 0:256]),
                     reads=[pid], writes=[uid])
                P.op("pool", lambda e, u_=u_, g=g: e.tensor_copy(out=u_[:, 0:16], in_=uH[:, g, i * 16:(i + 1) * 16]),
                     reads=[("uH", g)], writes=[(uid, "h")])
                rd = [uid, (uid, "h")]
                if combines:
                    combines.pop(0)()
                sbuf = [sS[g % 3], sS[(g + 1) % 3]]
                sids = [("sS", g % 3), ("sS", (g + 1) % 3)]
                P.op("pool", lambda e, u_=u_, o_=sbuf[0]: e.tensor_tensor(out=o_[:, 1:272], in0=u_[:, 1:272], in1=u_[:, 0:271], op=ALU.add),
                     reads=rd, writes=[sids[0]])
                cur = 0
                lo = 1
                for lvl in range(g):
                    sh = 2 << lvl
                    nlo = lo + sh
                    src_, dst_ = sbuf[cur], sbuf[1 - cur]
                    P.op("pool", lambda e, src_=src_, dst_=dst_, nlo=nlo, sh=sh: e.tensor_tensor(
                        out=dst_[:, nlo:272], in0=src_[:, nlo:272], in1=src_[:, nlo - sh:272 - sh], op=ALU.add),
                        reads=[sids[cur]], writes=[sids[1 - cur]])
                    cur = 1 - cur
                    lo = nlo
                S = sbuf[cur]
                sid = sids[cur]
                mx = mixT[g]
                mid = ("mix", g)

                def combine(g=g, S=S, sid=sid, u_=u_, rd=rd, mx=mx, mid=mid):
                    P.op("dve", lambda e: e.scalar_tensor_tensor(
                        out=mx, in0=u_[:, 16:272], scalar=-float(WINS[g]), in1=S[:, 16:272], op0=ALU.mult, op1=ALU.add),
                        reads=[sid] + rd, writes=[mid])
                    if i == 0:
                        P.op("dve", lambda e: e.tensor_tensor(out=fx[:, 0:16], in0=S[:, 16:32],
                                                              in1=cntfix[:, g * 16:(g + 1) * 16], op=ALU.mult),
                             reads=[sid, "cntfix"], writes=["fx"])
                        P.op("dve", lambda e: e.scalar_tensor_tensor(
                            out=mx[:, 0:16], in0=u_[:, 16:32], scalar=-float(WINS[g]), in1=fx[:, 0:16],
                            op0=ALU.mult, op1=ALU.add),
                            reads=["fx", mid] + rd, writes=[mid])
                combines.append(combine)

                def pool_tail(g=g, mx=mx, mid=mid):
                    pv2, pid2 = pp_slot()
                    P.op("pe", lambda e: e.matmul(out=pv2[:, 0:256], lhsT=wpool[:, g, :], rhs=mx, start=True, stop=True),
                         reads=["wpool", mid], writes=[pid2])
                    P.op("dve", lambda e: e.tensor_scalar(out=CAT[:, 4 + g, i * 256:(i + 1) * 256], in0=pv2[:, 0:256],
                                                          scalar1=pscw[:, g:g + 1], scalar2=None, op0=ALU.mult),
                         reads=[pid2, "pscw"], writes=[("cat", 4 + g, 2 * i), ("cat", 4 + g, 2 * i + 1)])
                deferred.append(pool_tail)
            while combines:
                combines.pop(0)()

        order = [("hist", 0)]
        for i in range(NOWN):
            order += [("ctx", i), ("own", i)]
        bufs = {}
        bufs[0] = a_begin(*order[0])
        a_tile(order[0][0], order[0][1], bufs[0], 0)
        bufs[1] = a_begin(*order[1])
        a_tile(order[1][0], order[1][1], bufs[1], 0)
        for gi, (name, c0, dst, d0) in enumerate(WGRP):
            P.op(("sp", "act")[gi % 2], lambda e, c0=c0: e.dma_start(out=WST[:, :, c0:c0 + 512], in_=w_in_v[:, :, c0:c0 + 512]),
                 writes=[("wst", name)], dma=True)
        ci = 0
        for name, c0, dst, d0 in WGRP:
            for kc in range(8):
                eng = "dve"
                ci += 1
                src = WST[:, kc, c0:c0 + 512]
                if eng == "act":
                    fn = lambda e, dst=dst, kc=kc, src=src, d0=d0: e.activation(
                        out=dst[:, kc, d0:d0 + 512], in_=src, func=AF.Copy, scale=gcol[:, kc:kc + 1])
                else:
                    fn = lambda e, dst=dst, kc=kc, src=src, d0=d0: e.tensor_scalar(
                        out=dst[:, kc, d0:d0 + 512], in0=src, scalar1=gcol[:, kc:kc + 1], scalar2=None, op0=ALU.mult)
                P.op(eng, fn, reads=[("wst", name), "gcol"], writes=[(name, kc)])
        fence([("wst", name) for name, _, _, _ in WGRP], kt_ids)
        for h in range(8):
            P.op("pool", lambda e, h=h: e.dma_start(out=KT[64:84, h, :], in_=kaug), writes=[("kaug", h)], dma=True)
        for n in range(len(order)):
            nxt = order[n + 1] if n + 1 < len(order) else None
            if nxt and n > 0:
                bufs[n + 1] = a_begin(*nxt)
                a_tile(nxt[0], nxt[1], bufs[n + 1], 0)
            late = a_tile(nxt[0], nxt[1], bufs[n + 1], 1, split=True) if nxt else None
            stage_b1(order[n][0], order[n][1], bufs[n])
            if late:
                late()
            stage_b1v(order[n][0], order[n][1], bufs[n])
            stage_b2(order[n][0], order[n][1], bufs[n])
        flush_deferred()

        p1_ids = (["hb", "fx", "cntfix", "wpool", "pscale", "pscw", "gcol"]
                  + [("sS", n_) for n_ in range(3)] + [("mix", g) for g in range(4)]
                  + [("xt", s) for s in range(2)] + [("hT", b, t) for b in range(2) for t in range(2)]
                  + [("ub", s) for s in range(2)] + [(("ub", s), "h") for s in range(2)]
                  + [("uH", g) for g in range(4)]
                  + [("ksum", h, ks) for h in range(8) for ks in range(16)]
                  + [(w_, kc) for w_ in ("wq", "wk", "wv", "wu") for kc in range(8)])
        cv.off = persist_end
        PT = [cv.bf(512) for _ in range(4)]
        atok = [cv.bf(1024).rearrange("p (t c) -> p t c", t=2) for _ in range(2)]
        rec = [cv.f32(2) for _ in range(2)]
        gm = cv.f32(128)
        m8 = cv.f32(64)
        stage = [cv.bf(128), cv.bf(128)]
        p3_ids = ([("PT", n) for n in range(4)] + [("atok", n) for n in range(2)] + [("rec", n) for n in range(2)]
                  + [("gmh", h) for h in range(8)] + [("m8", h) for h in range(8)] + [("stage", t_) for t_ in range(2)]
                  + [("cat", c, t) for c in range(4) for t in range(NT)] + [("wout", kc) for kc in range(8)])
        fence(p1_ids, p3_ids)
        for kc in range(8):
            P.op("pool", lambda e, kc=kc: e.dma_start(out=WOUT[:, kc, :], in_=w_out[kc * 128:(kc + 1) * 128, :]),
                 writes=[("wout", kc)], dma=True)

        units = []
        for i in range(NOWN):
            for h in range(H):
                wmax = int(np.floor((ALIBI_CUT / SLOPES[h] - 1.0) / 256.0 - 1e-9))
                for u in range(2 * i + 1):
                    gap = 2 * i - u
                    if gap <= wmax:
                        units.append((i, h, u))
                units.append((i, h, "diag"))
        st_ctr = [0]
        po_state = {}

        def emit_qk(unit):
            i, h, u = unit
            n = st_ctr[0] % 4
            nb_ = st_ctr[0] % 3
            st_ctr[0] += 1
            stv = psb[nb_]
            sid = fb_(nb_)
            qsl = QT[0:84, h, i * 256:(i + 1) * 256]
            qreads = [("qT", h, i), ("qM", 2 * i), ("qM", 2 * i + 1), "qaug"]
            if u != "diag":
                ks = u
                for c in range(2):
                    P.op("pe", lambda e, c=c: e.matmul(out=stv[:, c * 256:(c + 1) * 256],
                                                       lhsT=KT[0:84, h, ks * 256 + c * 128:ks * 256 + (c + 1) * 128],
                                                       rhs=qsl, start=True, stop=True),
                         reads=[("kT", h, ks), ("kaug", h)] + qreads, writes=sid)
                width = 512
            else:
                ks = 2 * i + 1
                k0 = KT[0:84, h, ks * 256:ks * 256 + 128]
                k1 = KT[0:84, h, ks * 256 + 128:ks * 256 + 256]
                rd = [("kT", h, ks), ("kaug", h)] + qreads
                P.op("pe", lambda e: e.matmul(out=stv[:, 0:128], lhsT=k0, rhs=qsl[:, 0:128], start=True, stop=False),
                     reads=rd, writes=sid)
                P.op("pe", lambda e: e.matmul(out=stv[:, 0:128], lhsT=ident, rhs=tri, start=False, stop=True),
                     reads=["ident", "tri"], writes=sid)
                P.op("pe", lambda e: e.matmul(out=stv[:, 128:256], lhsT=k0, rhs=qsl[:, 128:256], start=True, stop=True),
                     reads=rd, writes=sid)
                P.op("pe", lambda e: e.matmul(out=stv[:, 256:384], lhsT=k1, rhs=qsl[:, 128:256], start=True, stop=False),
                     reads=rd, writes=sid)
                P.op("pe", lambda e: e.matmul(out=stv[:, 256:384], lhsT=ident, rhs=tri, start=False, stop=True),
                     reads=["ident", "tri"], writes=sid)
                width = 384
            ptv = PT[n]
            P.op("act", lambda e: e.activation(out=ptv[:, 0:width], in_=stv[:, 0:width], func=AF.Exp),
                 reads=sid, writes=[("PT", n)])
            return n

        def emit_pv(unit, n):
            i, h, u = unit
            key = (i, h)
            if key not in po_state:
                b = len(po_state) % 2
                po_state[key] = dict(bank=b, started=False)
            stt = po_state[key]
            b = stt["bank"]
            pov = psb[4 + b]
            poid = fb_(4 + b)
            ptv = PT[n]
            if u != "diag":
                ks = u
                jobs = [(0, 0, ks * 2), (1, 128, ks * 2), (0, 256, ks * 2 + 1), (1, 384, ks * 2 + 1)]
                last = [False] * 4
            else:
                ks = 2 * i + 1
                jobs = [(0, 0, ks * 2), (1, 128, ks * 2), (1, 256, ks * 2 + 1)]
                last = [False, False, True]
            for (tt, c0, chunk), lst in zip(jobs, last):
                first = not stt["started"]
                stt["started"] = True
                P.op("pe", lambda e, tt=tt, c0=c0, chunk=chunk, first=first, lst=lst: e.matmul(
                    out=pov[:, tt * 65:(tt + 1) * 65], lhsT=ptv[:, c0:c0 + 128], rhs=VP[:, chunk, h, :],
                    start=first, stop=lst),
                    reads=[("PT", n), ("V", chunk), "vones"], writes=poid)
            if u == "diag":
                ab = i % 2
                rc = rec[b]
                P.op("dve", lambda e: e.reciprocal(out=rc, in_=pov[:, 0:130].rearrange("p (t c) -> p t c", t=2)[:, :, 64]),
                     reads=poid, writes=[("rec", b)])
                P.op("dve", lambda e: e.tensor_tensor(
                    out=atok[ab][:, :, h * 64:(h + 1) * 64],
                    in0=pov[:, 0:130].rearrange("p (t c) -> p t c", t=2)[:, :, 0:64],
                    in1=rc.unsqueeze(2).to_broadcast([128, 2, 64]), op=ALU.mult),
                    reads=poid + [("rec", b)], writes=[("atok", ab)])
                if h == H - 1:
                    def cat_tail(i=i, ab=ab):
                        trv, trid = tr3_bank()
                        for tt in range(2):
                            for cc in range(4):
                                P.op("pe", lambda e, tt=tt, cc=cc: e.transpose(
                                    out=trv[:, cc * 256 + tt * 128:cc * 256 + (tt + 1) * 128],
                                    in_=atok[ab][:, tt, cc * 128:(cc + 1) * 128], identity=ident),
                                    reads=[("atok", ab), "ident"], writes=trid)
                        P.op("dve", lambda e: e.tensor_copy(out=CAT[:, 0:4, i * 256:(i + 1) * 256],
                                                            in_=trv.rearrange("p (c s) -> p c s", c=4)),
                             reads=trid, writes=[("cat", c, 2 * i + t_) for c in range(4) for t_ in range(2)])
                    cat_pending.append([6, cat_tail])

        tr3_ctr = [0]
        cat_pending = []

        def cat_tick(force=False):
            for ent in list(cat_pending):
                ent[0] -= 1
                if ent[0] <= 0 or force:
                    cat_pending.remove(ent)
                    ent[1]()

        def tr3_bank():
            n = tr3_ctr[0] % 2
            tr3_ctr[0] += 1
            return psb[6 + n][:, :].bitcast(BF16), fb_(6 + n)

        gate_pending = {}
        gate_chains = {}

        def gate_chain(i, tt):
            gate_chains[i][tt]()

        def gate_front(i):
            tails = []
            gid = fb_(3)
            for tt in range(2):
                t = 2 * i + tt
                gvf = psb[3][:, tt * 128:(tt + 1) * 128]
                for h in range(8):
                    P.op("pe", lambda e, gvf=gvf, h=h, t=t: e.matmul(
                        out=gvf[:, h * 16:(h + 1) * 16], lhsT=QT[0:64, h, t * 128:(t + 1) * 128],
                        rhs=kmT[0:64, h * 16:(h + 1) * 16], start=True, stop=True),
                        reads=[("qT", h, i)] + [("kmT", s_) for s_ in range(16)], writes=gid)
            chains = []
            for tt in range(2):
              def chain(tt=tt):
                t = 2 * i + tt
                gvf = psb[3][:, tt * 128:(tt + 1) * 128]
                gmv = gm.rearrange("p (h s) -> p h s", h=8)
                elb = elig[:, i, :].unsqueeze(1).to_broadcast([128, 8, 16])
                owb = owntab[:, i, :].unsqueeze(1).to_broadcast([128, 8, 16])
                stg = stage[tt]
                P.op("dve", lambda e, gvf=gvf, gmv=gmv, elb=elb: e.tensor_tensor(
                    out=gmv, in0=gvf[:, 0:128].rearrange("p (h s) -> p h s", h=8), in1=elb, op=ALU.add),
                    reads=gid + ["elig"], writes=GMH)
                for h in range(8):
                    P.op("dve", lambda e, h=h: e.max(out=m8[:, h * 8:(h + 1) * 8], in_=gm[:, h * 16:(h + 1) * 16]),
                         reads=[("gmh", h)], writes=[("m8", h)])
                for h in range(8):
                    P.op("dve", lambda e, h=h: e.tensor_scalar(
                        out=gm[:, h * 16:(h + 1) * 16], in0=gm[:, h * 16:(h + 1) * 16],
                        scalar1=m8[:, h * 8 + 2:h * 8 + 3], scalar2=-BIG, op0=ALU.is_lt, op1=ALU.mult),
                        reads=[("gmh", h), ("m8", h)], writes=[("gmh", h)])
                P.op("dve", lambda e, gmv=gmv, elb=elb: e.tensor_tensor(out=gmv, in0=gmv, in1=elb, op=ALU.add),
                     reads=["elig"] + GMH, writes=GMH)
                P.op("dve", lambda e, gmv=gmv, owb=owb, stg=stg: e.tensor_tensor(
                    out=stg.rearrange("p (h s) -> p h s", h=8), in0=gmv, in1=owb, op=ALU.max),
                    reads=GMH + ["owntab"], writes=[("stage", tt)])

                def tail(t=t, tt=tt, stg=stg):
                    trv, trid = tr3_bank()
                    for h in range(8):
                        P.op("pe", lambda e, h=h: e.transpose(out=trv[0:16, h * 128:(h + 1) * 128],
                                                              in_=stg[:, h * 16:(h + 1) * 16], identity=ident),
                             reads=[("stage", tt), "ident"], writes=trid)
                    P.op("dve", lambda e: e.tensor_copy(out=QT[64:80, :, t * 128:(t + 1) * 128],
                                                        in_=trv[0:16, :].rearrange("p (h s) -> p h s", h=8)),
                         reads=trid, writes=[("qM", t)])
                tails.append(tail)
              chains.append(chain)
            gate_pending[i] = tails
            gate_chains[i] = chains

        def gate_tail(i):
            for f_ in gate_pending.pop(i):
                f_()

        gate_front(0)
        gate_chain(0, 0)
        gate_chain(0, 1)
        gate_tail(0)
        pend = []
        prev_ih = None
        for unit in units:
            ui, uh, uu = unit
            if (ui, uh) != prev_ih and ui + 1 < NOWN:
                if uh == 3:
                    gate_front(ui + 1)
                elif uh == 4:
                    gate_chain(ui + 1, 0)
                elif uh == 5:
                    gate_chain(ui + 1, 1)
                elif uh == 7:
                    gate_tail(ui + 1)
            prev_ih = (ui, uh)
            n = emit_qk(unit)
            pend.append((unit, n))
            if len(pend) > 2:
                emit_pv(*pend.pop(0))
            cat_tick()
        while pend:
            emit_pv(*pend.pop(0))
        cat_tick(force=True)

        if debug:
            P.op("sp", lambda e: e.dma_start(out=dbg["qt"][0:84, :], in_=RBC[0:84, 16640:33024]),
                 reads=[("qT", h, i) for h in range(8) for i in range(8)] + [("qM", t) for t in range(NT)] + ["qaug"],
                 dma=True)
            P.op("sp", lambda e: e.dma_start(out=dbg["kt"][0:84, :], in_=RA[0:84, :]), reads=kt_ids, dma=True)
            P.op("sp", lambda e: e.dma_start(out=dbg["vp"], in_=RBC[:, 0:16640]),
                 reads=[("V", c) for c in range(32)] + ["vones"], dma=True)

        if debug:
            P.op("sp", lambda e: e.dma_start(out=dbg["cat"], in_=RD[:, :]),
                 reads=[("cat", c, t) for c in range(8) for t in range(NT)], dma=True)

        p3_old = ([("PT", n) for n in range(4)] + [("atok", n) for n in range(2)] + [("rec", n) for n in range(2)]
                  + [("gmh", h) for h in range(8)] + [("m8", h) for h in range(8)] + [("stage", t_) for t_ in range(2)]
                  + [("kmT", ks) for ks in range(16)] + ["elig", "owntab"]
                  + kt_ids + [("V", c) for c in range(32)] + ["vones", "qaug"]
                  + [("qT", h, i) for h in range(8) for i in range(8)] + [("qM", t) for t in range(NT)])
        cv.off = persist_end
        gb = cv.f32(1024)
        hb4 = [cv.bf(1024), cv.bf(1024), cv.bf(1024)]
        actT = [cv.bf(2048).rearrange("p (f s) -> p f s", f=8) for _ in range(2)]
        rtmp = [cv.f32(256) for _ in range(2)]
        xr = [cv.f32(1024), cv.f32(1024)]
        p4_ids = (["gb", ("hb4", 0), ("hb4", 1), ("hb4", 2)] + [("actT", n, fb) for n in range(2) for fb in range(8)] + [("rtmp", n) for n in range(2)]
                  + [("xr", n) for n in range(2)] + [("xm", t, c) for t in range(NT) for c in range(2)]
                  + [("wup", s, kc) for s in range(2) for kc in range(8)]
                  + [("wdn", s, kc) for s in range(2) for kc in range(8)])
        fence(p3_old, p4_ids)

        def load_ffn(fg):
            s = fg % 2
            wu, wd = ring(s)
            for kc in range(8):
                P.op("pool", lambda e, kc=kc: e.dma_start(
                    out=wu[:, kc, :], in_=w_up[kc * 128:(kc + 1) * 128, fg * 1024:(fg + 1) * 1024]),
                    writes=[("wup", s, kc)], dma=True)
            for fb in range(8):
                P.op("pool", lambda e, fb=fb: e.dma_start(
                    out=wd[:, fb, :], in_=w_down[fg * 1024 + fb * 128:fg * 1024 + (fb + 1) * 128, :]),
                    writes=[("wdn", s, fb)], dma=True)

        load_ffn(0)
        load_ffn(1)
        P.op("sp", lambda e: e.dma_start(out=gb, in_=g_mlp.partition_broadcast(128)), writes=["gb"], dma=True)

        tr4_ctr = [0]

        def wout_mm(t):
            slot = t % 2
            P.op("sp", lambda e: e.dma_start(out=xr[slot], in_=x_own[t * 128:(t + 1) * 128, :]),
                 writes=[("xr", slot)], dma=True)
            n = t % 3
            for ch in range(2):
                bank = psb[2 * n + ch]
                bid = fb_(2 * n + ch)
                for kc in range(8):
                    P.op("pe", lambda e, bank=bank, kc=kc, ch=ch: e.matmul(
                        out=bank[:, :], lhsT=CAT[:, kc, t * 128:(t + 1) * 128], rhs=WOUT[:, kc, ch * 512:(ch + 1) * 512],
                        start=(kc == 0), stop=(kc == 7)),
                        reads=[("cat", kc, t), ("wout", kc)], writes=bid)
                P.op("dve", lambda e, bank=bank, ch=ch: e.tensor_tensor(
                    out=XM[:, t, ch * 512:(ch + 1) * 512], in0=bank[:, :], in1=xr[slot][:, ch * 512:(ch + 1) * 512], op=ALU.add),
                    reads=bid + [("xr", slot)], writes=[("xm", t, ch)])
            col = 34 + t
            norm_rs(col, XM[:, t, :], [("xm", t, 0), ("xm", t, 1)], hb4[t % 3], ("hb4", t % 3))
            P.op("dve", lambda e: e.scalar_tensor_tensor(out=hb4[t % 3], in0=XM[:, t, :], scalar=rs[:, col:col + 1], in1=gb,
                                                         op0=ALU.mult, op1=ALU.mult),
                 reads=[("xm", t, 0), ("xm", t, 1), ("rs", col), "gb"], writes=[("hb4", t % 3)])

        def wout_tail(t):
            k = tr4_ctr[0] % 2
            tr4_ctr[0] += 1
            trv = psb[6 + k][:, :].bitcast(BF16)
            trid = fb_(6 + k)
            for kc in range(8):
                P.op("pe", lambda e, kc=kc: e.transpose(out=trv[:, kc * 128:(kc + 1) * 128],
                                                        in_=hb4[t % 3][:, kc * 128:(kc + 1) * 128], identity=ident),
                     reads=[("hb4", t % 3), "ident"], writes=trid)
            P.op("act", lambda e: e.copy(out=CAT[:, :, t * 128:(t + 1) * 128], in_=trv.rearrange("p (k s) -> p k s", k=8)),
                 reads=trid, writes=[("cat", c, t) for c in range(8)])

        for t in range(NT):
            wout_mm(t)
            if t >= 2:
                wout_tail(t - 2)
        ffn_inject = {0: [lambda: wout_tail(NT - 2)], 1: [lambda: wout_tail(NT - 1)]}

        pu_ctr = [0]
        pd_ctr = [0]
        act_ctr = [0]
        rt_ctr = [0]

        def ffn_up(fg, tp):
            s = fg % 2
            wu, _ = ring(s)
            ab = act_ctr[0] % 2
            act_ctr[0] += 1
            for fb in range(8):
                n = pu_ctr[0] % 4
                pu_ctr[0] += 1
                pv = psb[n][:, 0:256]
                pid = ("ps", n)
                for kc in range(8):
                    P.op("pe", lambda e, pv=pv, kc=kc, fb=fb: e.matmul(
                        out=pv, lhsT=wu[:, kc, fb * 128:(fb + 1) * 128], rhs=CAT[:, kc, tp * 256:(tp + 1) * 256],
                        start=(kc == 0), stop=(kc == 7)),
                        reads=[("wup", s, kc), ("cat", kc, 2 * tp), ("cat", kc, 2 * tp + 1)], writes=[pid])
                r = rt_ctr[0] % 2
                rt_ctr[0] += 1
                P.op("act", lambda e, pv=pv, r=r: e.activation(out=rtmp[r], in_=pv, func=AF.Relu),
                     reads=[pid], writes=[("rtmp", r)])
                P.op("dve", lambda e, r=r, fb=fb, ab=ab: e.tensor_tensor(out=actT[ab][:, fb, :], in0=rtmp[r], in1=rtmp[r],
                                                                       op=ALU.mult),
                     reads=[("rtmp", r)], writes=[("actT", ab, fb)])
            return ab

        def ffn_down(fg, tp, ab):
            s = fg % 2
            _, wd = ring(s)
            for tt in range(2):
                t = 2 * tp + tt
                for ch in range(2):
                    n = pd_ctr[0] % 4
                    pd_ctr[0] += 1
                    bank = psb[4 + n]
                    bid = fb_(4 + n)
                    for fb in range(8):
                        P.op("pe", lambda e, bank=bank, fb=fb, tt=tt, ch=ch: e.matmul(
                            out=bank[:, :], lhsT=actT[ab][:, fb, tt * 128:(tt + 1) * 128],
                            rhs=wd[:, fb, ch * 512:(ch + 1) * 512], start=(fb == 0), stop=(fb == 7)),
                            reads=[("actT", ab, fb), ("wdn", s, fb)], writes=bid)
                    P.op("dve", lambda e, bank=bank, t=t, ch=ch: e.tensor_tensor(
                        out=XM[:, t, ch * 512:(ch + 1) * 512], in0=bank[:, :], in1=XM[:, t, ch * 512:(ch + 1) * 512],
                        op=ALU.add),
                        reads=bid + [("xm", t, ch)], writes=[("xm", t, ch)])

        out_ops = []

        def final_tile(t):
            col = 50 + t
            norm_rs(col, XM[:, t, :], [("xm", t, 0), ("xm", t, 1)], hb4[t % 3], ("hb4", t % 3))
            slot = t % 2
            P.op("dve", lambda e: e.scalar_tensor_tensor(out=xr[slot], in0=XM[:, t, :], scalar=rs[:, col:col + 1], in1=gb,
                                                         op0=ALU.mult, op1=ALU.mult),
                 reads=[("xm", t, 0), ("xm", t, 1), ("rs", col), "gb"], writes=[("xr", slot)])
            out_ops.append(P.op("sp", lambda e: e.dma_start(out=y[t * 128:(t + 1) * 128, :], in_=xr[slot]),
                                reads=[("xr", slot)], dma=True))

        seq = [(fg, tp) for fg in range(4) for tp in range(8)]
        prev = None
        loaded = 2
        gfin_loaded = False
        for idx, (fg, tp) in enumerate(seq):
            ab = ffn_up(fg, tp)
            for f_ in ffn_inject.pop(idx, []):
                f_()
            if prev is not None:
                pfg, ptp, pab = prev
                ffn_down(pfg, ptp, pab)
                if ptp == 7 and pfg + 2 < 4:
                    load_ffn(pfg + 2)
                if pfg == 3:
                    if not gfin_loaded:
                        P.op("sp", lambda e: e.dma_start(out=gb, in_=g_fin.partition_broadcast(128)), writes=["gb"], dma=True)
                        gfin_loaded = True
                    final_tile(2 * ptp)
                    final_tile(2 * ptp + 1)
            prev = (fg, tp, ab)
        pfg, ptp, pab = prev
        ffn_down(pfg, ptp, pab)
        final_tile(2 * ptp)
        final_tile(2 * ptp + 1)

        dbg_ops = [o for o in P.ops if o.dma and o.eng == "sp" and not o.writes and o not in out_ops]
        P.op("sp", lambda e: None, after=out_ops + dbg_ops)
        stats = P.emit(st)
    return nc, stats


def _core_tables(r):
    bf = ml_dtypes.bfloat16
    own_g = [2 * i + r for i in range(NOWN)]
    ctx_g = [(2 * i - 1 + r) % NBLK for i in range(NOWN)]
    slot_g = []
    for i in range(NOWN):
        slot_g += [ctx_g[i], own_g[i]]
    kaug = np.zeros((20, 4096), np.float32)
    for ks in range(16):
        sl = slice(ks * 256, (ks + 1) * 256)
        kaug[ks, sl] = 1.0
        kaug[16, sl] = 1.0
        kaug[17, sl] = 1.0
        kaug[18, sl] = np.arange(256)
        kaug[19, sl] = slot_g[ks]
    qaug = np.zeros((4, H, NTOK), np.float32)
    for h in range(H):
        s = SLOPES[h]
        for i in range(NOWN):
            sl = slice(i * 256, (i + 1) * 256)
            qaug[0, h, sl] = -s * np.arange(256)
            qaug[1, h, sl] = -s * 256.0 * own_g[i]
            qaug[2, h, sl] = s
            qaug[3, h, sl] = s * 256.0
    elig = np.zeros((128, NOWN, 16), np.float32)
    owntab = np.full((128, NOWN, 16), -3.0 * BIG, np.float32)
    for i in range(NOWN):
        for ks in range(16):
            elig[:, i, ks] = 0.0 if slot_g[ks] < own_g[i] else -BIG
        owntab[:, i, 2 * i + 1] = 0.0
    tri = np.where(np.arange(128)[:, None] <= np.arange(128)[None, :], 0.0, -BIG).astype(np.float32)
    ident = np.eye(128, dtype=np.float32)
    cntfix = np.zeros((128, 4, 16), np.float32)
    for g, w in enumerate(WINS):
        pos = own_g[0] * 256 + np.arange(16)
        cntfix[:, g, :] = float(w) / np.minimum(pos + 1, w)
    return dict(kaug=kaug.astype(bf), qaug=qaug.reshape(4, H * NTOK).astype(bf),
                elig=elig.reshape(128, -1).astype(bf), owntab=owntab.reshape(128, -1).astype(bf),
                tri=tri.astype(bf), ident=ident.astype(bf), cntfix=cntfix.reshape(128, 64)), own_g, ctx_g


_CACHE = {}


def kernel(x, norm_mix, w_in, w_pool, pool_scale, w_out, norm_mlp, w_up, w_down, norm_final, _debug=False):
    x = np.ascontiguousarray(np.asarray(x, dtype=np.float32))
    f = lambda a: np.ascontiguousarray(np.asarray(a, dtype=np.float32))
    key = ("nc", bool(_debug))
    if key not in _CACHE:
        _CACHE[key] = build_program(debug=_debug)
    nc, stats = _CACHE[key]
    shared = dict(
        w_in=f(w_in)[0], w_out=f(w_out)[0], w_up=f(w_up)[0], w_down=f(w_down)[0],
        w_pool=f(w_pool)[0].reshape(512, 128),
        pscale_t=np.ascontiguousarray(f(pool_scale)[0].reshape(4, 128).T),
        gmix_t=np.ascontiguousarray(f(norm_mix)[0].reshape(8, 128).T),
        g_mlp=f(norm_mlp)[0], g_fin=f(norm_final),
    )
    in_maps = []
    meta = []
    for c in range(NCORES):
        b, r = divmod(c, 2)
        tabs, own_g, ctx_g = _core_tables(r)
        xb = x[b].reshape(NBLK, BLK, D)
        x_own = np.ascontiguousarray(xb[own_g].reshape(NTOK, D))
        x_ctx = np.ascontiguousarray(xb[ctx_g].reshape(NTOK, D))
        x_hist = np.zeros((NOWN, 16, D), np.float32)
        for i, g in enumerate(own_g):
            if g > 0:
                x_hist[i] = x[b, g * BLK - 16:g * BLK]
        m = dict(shared)
        m.update(tabs)
        m.update(x_own=x_own, x_ctx=x_ctx, x_hist=x_hist.reshape(128, D))
        in_maps.append(m)
        meta.append((b, own_g))
    res = run_bass_kernel_spmd(nc, in_maps, core_ids=list(range(NCORES)))
    out = np.empty((NBATCH, SEQ, D), np.float32)
    for c in range(NCORES):
        b, own_g = meta[c]
        yb = np.asarray(res.results[c]["y"]).reshape(NOWN, BLK, D)
        ob = out[b].reshape(NBLK, BLK, D)
        for i, g in enumerate(own_g):
            ob[g] = yb[i]
    if _debug:
        return out, res
    return out
```
